# Optimizing a Trainium2 kernel written in Bass

```python
import jax
import jax.numpy as jnp
from jax import lax
import numpy as np

D_MODEL = 1024
BATCH = 4
SEQ = 4096
DEPTH = 1

ATTN_GROUPS = 3
ATTN_HEADS_PER_GROUP = 4
ATTN_HEADS = ATTN_GROUPS * ATTN_HEADS_PER_GROUP
ATTN_HEAD_DIM = 128
ATTN_WIDTH = ATTN_HEADS * ATTN_HEAD_DIM
ATTN_OUT_WIDTH = ATTN_HEADS_PER_GROUP * ATTN_HEAD_DIM
ATTN_PATTERNS = ((128, 1), (512, 4), (2048, 16))
ALIBI_MAX_EXP = 8.0
NEG_INF = -1e30
RET_QK_DIM = 256
RET_HEADS = D_MODEL // RET_QK_DIM
RET_V_DIM = 2 * RET_QK_DIM
RET_QK_WIDTH = RET_HEADS * RET_QK_DIM
RET_V_WIDTH = RET_HEADS * RET_V_DIM
RET_CHUNK = 128
IN_SIZES = (ATTN_WIDTH, ATTN_WIDTH, ATTN_WIDTH, RET_QK_WIDTH, RET_QK_WIDTH,
            RET_V_WIDTH, RET_V_WIDTH, D_MODEL, D_MODEL)
N_GROUPS = 4
EXPERTS_PER_GROUP = 8
N_EXPERTS = N_GROUPS * EXPERTS_PER_GROUP
TOP_K_FINE = 2
EXPERT_FF = D_MODEL // 2
MOE_BLOCK = 128
DEEPNORM_ALPHA = (2.0 * DEPTH) ** 0.25
DEEPNORM_BETA = (8.0 * DEPTH) ** -0.25
LN_EPS = 1e-5
ADA_STD = 0.5

kernel_name = 'hybrid_dilated_retention_hmoe_block'


def _layer_norm(x):
    xf = x.astype(jnp.float32)
    mu = jnp.mean(xf, axis=-1, keepdims=True)
    var = jnp.mean(jnp.square(xf - mu), axis=-1, keepdims=True)
    return ((xf - mu) * lax.rsqrt(var + LN_EPS)).astype(x.dtype)


def _layer_norm_affine(x, gain, bias):
    return _layer_norm(x) * gain + bias


def _split_columns(proj, sizes):
    out, start = [], 0
    for n in sizes:
        out.append(proj[..., start:start + n])
        start += n
    return out


def _alibi_slopes(n_heads):
    return jnp.exp2(-ALIBI_MAX_EXP * jnp.arange(1, n_heads + 1, dtype=jnp.float32) / n_heads)


def _dilated_window_attention(q, k, v, slopes, window, dilation):
    b, s, h, dh = q.shape
    half = window // (2 * dilation)
    blk = half
    n_sub = s // dilation
    nb = -(-n_sub // blk)
    n_pad = nb * blk

    def to_sub(t):
        t = t.reshape(b, n_sub, dilation, h, dh).transpose(0, 2, 3, 1, 4)
        return jnp.pad(t, ((0, 0), (0, 0), (0, 0), (0, n_pad - n_sub), (0, 0)))

    def to_band(t):
        tp = jnp.pad(to_sub(t), ((0, 0), (0, 0), (0, 0), (blk, blk), (0, 0)))
        tp = tp.reshape(b, dilation, h, nb + 2, blk, dh)
        return jnp.concatenate([tp[:, :, :, :nb], tp[:, :, :, 1:nb + 1], tp[:, :, :, 2:]], axis=4)

    qb = to_sub(q).reshape(b, dilation, h, nb, blk, dh)
    kb, vb = to_band(k), to_band(v)
    q_idx = jnp.arange(nb)[:, None] * blk + jnp.arange(blk)[None, :]
    k_idx = (jnp.arange(nb)[:, None] - 1) * blk + jnp.arange(3 * blk)[None, :]
    delta = k_idx[:, None, :] - q_idx[:, :, None]
    valid = (jnp.abs(delta) <= half) & (k_idx[:, None, :] >= 0) & (k_idx[:, None, :] < n_sub)
    dist = (dilation * jnp.abs(delta)).astype(jnp.float32)
    bias = -slopes[:, None, None, None] * dist[None]
    scores = jnp.einsum('brhnqd,brhnkd->brhnqk', qb, kb).astype(jnp.float32) * (dh ** -0.5)
    scores = jnp.where(valid, scores + bias, NEG_INF)
    m = jnp.max(scores, axis=-1, keepdims=True)
    lse = m + jnp.log(jnp.sum(jnp.exp(scores - m), axis=-1, keepdims=True))
    probs = jnp.exp(scores - lse)
    out = jnp.einsum('brhnqk,brhnkd->brhnqd', probs.astype(v.dtype), vb)
    out = out.reshape(b, dilation, h, n_pad, dh)[:, :, :, :n_sub]
    out = out.transpose(0, 3, 1, 2, 4).reshape(b, s, h, dh)
    lse = lse[..., 0].reshape(b, dilation, h, n_pad)[..., :n_sub]
    lse = lse.transpose(0, 3, 1, 2).reshape(b, s, h)
    return out, lse


def _retention_one_direction(q, k, v, log_gamma, include_diag):
    b, h, s, dk = q.shape
    dv = v.shape[-1]
    c = RET_CHUNK
    nc = s // c

    def chunks(t):
        return t.reshape(b, h, nc, c, t.shape[-1]).transpose(2, 0, 1, 3, 4)

    pos = jnp.arange(c, dtype=jnp.float32)
    delta = pos[:, None] - pos[None, :]
    lower = delta >= 0 if include_diag else delta > 0
    lg = log_gamma[:, None, None]
    decay_mat = jnp.where(lower[None], jnp.exp(lg * jnp.maximum(delta, 0.0)[None]), 0.0)
    key_decay = jnp.exp(log_gamma[:, None] * (c - 1 - pos)[None])[None, :, :, None]
    query_decay = jnp.exp(log_gamma[:, None] * (pos + 1)[None])[None, :, :, None]
    chunk_decay = jnp.exp(log_gamma * c)[None, :, None, None]

    def step(state, inp):
        qi, ki, vi = inp
        inner = jnp.einsum('bhnd,bhmd->bhnm', qi, ki) * decay_mat
        y = jnp.einsum('bhnm,bhme->bhne', inner, vi)
        y = y + jnp.einsum('bhnd,bhde->bhne', qi, state) * query_decay
        state = state * chunk_decay + jnp.einsum('bhmd,bhme->bhde', ki * key_decay, vi)
        return state, y

    state0 = jnp.zeros((b, h, dk, dv), jnp.float32)
    _, ys = lax.scan(step, state0, (chunks(q), chunks(k), chunks(v)))
    return ys.transpose(1, 2, 0, 3, 4).reshape(b, h, s, dv)


def _bidirectional_retention(q, k, v, logit_fwd, logit_bwd):
    q, k, v = q.astype(jnp.float32), k.astype(jnp.float32), v.astype(jnp.float32)
    lg_f = jax.nn.log_sigmoid(logit_fwd.astype(jnp.float32))
    lg_b = jax.nn.log_sigmoid(logit_bwd.astype(jnp.float32))
    fwd = _retention_one_direction(q, k, v, lg_f, True)
    flip = lambda t: jnp.flip(t, axis=2)
    bwd = flip(_retention_one_direction(flip(q), flip(k), flip(v), lg_b, False))
    return fwd + bwd


def _token_mixing(u, w_in, w_attn_out, ret_decay_fwd, ret_decay_bwd, ret_gn_gain, w_ret_out, w_out):
    b, s, _ = u.shape
    proj = u @ w_in
    aq, ak, av, rq, rk, rv, rg, gate_a, gate_r = _split_columns(proj, IN_SIZES)

    heads = lambda t: t.reshape(b, s, ATTN_HEADS, ATTN_HEAD_DIM)
    aq, ak, av = heads(aq), heads(ak), heads(av)
    slopes = _alibi_slopes(ATTN_HEADS)
    outs, lses = [], []
    for g, (window, dilation) in enumerate(ATTN_PATTERNS):
        sl = slice(g * ATTN_HEADS_PER_GROUP, (g + 1) * ATTN_HEADS_PER_GROUP)
        o, l = _dilated_window_attention(aq[:, :, sl], ak[:, :, sl], av[:, :, sl], slopes[sl], window, dilation)
        outs.append(o)
        lses.append(l)
    dil_w = jax.nn.softmax(jnp.stack(lses), axis=0)
    attn = jnp.einsum('gbsh,gbshd->bshd', dil_w.astype(u.dtype), jnp.stack(outs))
    branch_a = attn.reshape(b, s, ATTN_OUT_WIDTH) @ w_attn_out

    rheads = lambda t, d: t.reshape(b, s, RET_HEADS, d).transpose(0, 2, 1, 3)
    ret = _bidirectional_retention(rheads(rq, RET_QK_DIM), rheads(rk, RET_QK_DIM) * (RET_QK_DIM ** -0.5),
                                   rheads(rv, RET_V_DIM), ret_decay_fwd, ret_decay_bwd)
    ret = _layer_norm(ret).transpose(0, 2, 1, 3).reshape(b, s, RET_V_WIDTH).astype(u.dtype) * ret_gn_gain
    branch_r = (jax.nn.silu(rg) * ret) @ w_ret_out

    merged = jax.nn.sigmoid(gate_a) * branch_a + jax.nn.sigmoid(gate_r) * branch_r
    return merged @ w_out


def _hierarchical_moe(u, w_coarse, b_coarse, w_fine, b_fine, w1, w3, w2):
    b, s, d = u.shape
    t = b * s
    xt = u.reshape(t, d)
    tok = jnp.arange(t)
    coarse = (xt @ w_coarse).astype(jnp.float32) + b_coarse
    p_coarse = jax.nn.softmax(coarse, axis=-1)
    _, g_top = lax.top_k(coarse, 1)
    g_sel = g_top[:, 0]
    p_group = p_coarse[tok, g_sel]
    fine_all = jnp.einsum('td,gde->tge', xt, w_fine).astype(jnp.float32) + b_fine
    fine = fine_all[tok, g_sel]
    f_val, f_idx = lax.top_k(fine, TOP_K_FINE)
    gate = p_group[:, None] * jax.nn.softmax(f_val, axis=-1)
    expert = g_sel[:, None] * EXPERTS_PER_GROUP + f_idx
    n_assign = t * TOP_K_FINE
    e_flat = expert.reshape(n_assign)
    order = jnp.argsort(e_flat)
    e_sorted = e_flat[order]
    tok_sorted = jnp.repeat(tok, TOP_K_FINE)[order]
    gate_sorted = gate.reshape(n_assign)[order]
    counts = jnp.bincount(e_flat, length=N_EXPERTS)
    padded = (counts + MOE_BLOCK - 1) // MOE_BLOCK * MOE_BLOCK
    start = jnp.cumsum(counts) - counts
    end_p = jnp.cumsum(padded)
    start_p = end_p - padded
    dest = start_p[e_sorted] + jnp.arange(n_assign) - start[e_sorted]
    n_rows = (-(-n_assign // MOE_BLOCK) + N_EXPERTS) * MOE_BLOCK
    n_blocks = n_rows // MOE_BLOCK
    rows = jnp.zeros((n_rows, d), u.dtype).at[dest].set(xt[tok_sorted])
    block_expert = jnp.minimum(
        jnp.searchsorted(end_p, jnp.arange(n_blocks) * MOE_BLOCK, side='right'), N_EXPERTS - 1)

    def expert_block(args):
        xb, e = args
        hdn = jax.nn.silu(xb @ w1[e]) * (xb @ w3[e])
        return hdn @ w2[e]

    y_rows = lax.map(expert_block, (rows.reshape(n_blocks, MOE_BLOCK, d), block_expert)).reshape(n_rows, d)
    y_assign = y_rows[dest] * gate_sorted[:, None].astype(u.dtype)
    out = jnp.zeros((t, d), u.dtype).at[tok_sorted].add(y_assign)
    return out.reshape(b, s, d)


def setup_inputs(seed: int = 0) -> dict:
    key = jax.random.key(seed)
    ks = jax.random.split(key, 32)
    f32 = jnp.float32
    beta = DEEPNORM_BETA

    def nrm(k, shape, scale):
        return jax.random.normal(k, shape, f32) * scale

    x = nrm(ks[0], (BATCH, SEQ, D_MODEL), 1.0)
    c = nrm(ks[1], (BATCH, D_MODEL), 1.0)
    w_ada = nrm(ks[2], (DEPTH, D_MODEL, 6 * D_MODEL), ADA_STD * D_MODEL ** -0.5)
    b_ada = nrm(ks[3], (DEPTH, 6 * D_MODEL), 0.01)
    in_scales = (1.0, 1.0, beta, 1.0, 1.0, beta, 1.0, 1.0, 1.0)
    w_in = jnp.concatenate(
        [nrm(ks[4 + i], (DEPTH, D_MODEL, n), sc * D_MODEL ** -0.5)
         for i, (n, sc) in enumerate(zip(IN_SIZES, in_scales))], axis=-1)
    w_attn_out = nrm(ks[13], (DEPTH, ATTN_OUT_WIDTH, D_MODEL), beta * ATTN_OUT_WIDTH ** -0.5)
    gamma0 = 1.0 - jnp.exp2(-5.0 - jnp.arange(RET_HEADS, dtype=f32))
    base_logit = jnp.log(gamma0) - jnp.log1p(-gamma0)
    ret_decay_fwd = base_logit + nrm(ks[14], (DEPTH, RET_HEADS), 0.05)
    ret_decay_bwd = base_logit + nrm(ks[15], (DEPTH, RET_HEADS), 0.05)
    ret_gn_gain = 1.0 + nrm(ks[16], (DEPTH, RET_V_WIDTH), 0.05)
    w_ret_out = nrm(ks[17], (DEPTH, RET_V_WIDTH, D_MODEL), beta * RET_V_WIDTH ** -0.5)
    w_out = nrm(ks[18], (DEPTH, D_MODEL, D_MODEL), beta * D_MODEL ** -0.5)
    ln1_gain = 1.0 + nrm(ks[19], (DEPTH, D_MODEL), 0.05)
    ln1_bias = nrm(ks[20], (DEPTH, D_MODEL), 0.01)
    w_coarse = nrm(ks[21], (DEPTH, D_MODEL, N_GROUPS), D_MODEL ** -0.5)
    b_coarse = nrm(ks[22], (DEPTH, N_GROUPS), 0.01)
    w_fine = nrm(ks[23], (DEPTH, N_GROUPS, D_MODEL, EXPERTS_PER_GROUP), D_MODEL ** -0.5)
    b_fine = nrm(ks[24], (DEPTH, N_GROUPS, EXPERTS_PER_GROUP), 0.01)
    w1 = nrm(ks[25], (DEPTH, N_EXPERTS, D_MODEL, EXPERT_FF), beta * D_MODEL ** -0.5)
    w3 = nrm(ks[26], (DEPTH, N_EXPERTS, D_MODEL, EXPERT_FF), beta * D_MODEL ** -0.5)
    w2 = nrm(ks[27], (DEPTH, N_EXPERTS, EXPERT_FF, D_MODEL), beta * EXPERT_FF ** -0.5)
    ln2_gain = 1.0 + nrm(ks[28], (DEPTH, D_MODEL), 0.05)
    ln2_bias = nrm(ks[29], (DEPTH, D_MODEL), 0.01)
    return {'x': x, 'c': c, 'w_ada': w_ada, 'b_ada': b_ada, 'w_in': w_in, 'w_attn_out': w_attn_out,
            'ret_decay_fwd': ret_decay_fwd, 'ret_decay_bwd': ret_decay_bwd, 'ret_gn_gain': ret_gn_gain,
            'w_ret_out': w_ret_out, 'w_out': w_out, 'ln1_gain': ln1_gain, 'ln1_bias': ln1_bias,
            'w_coarse': w_coarse, 'b_coarse': b_coarse, 'w_fine': w_fine, 'b_fine': b_fine,
            'w1': w1, 'w3': w3, 'w2': w2, 'ln2_gain': ln2_gain, 'ln2_bias': ln2_bias}


def reference(x, c, w_ada, b_ada, w_in, w_attn_out, ret_decay_fwd, ret_decay_bwd, ret_gn_gain,
              w_ret_out, w_out, ln1_gain, ln1_bias, w_coarse, b_coarse, w_fine, b_fine,
              w1, w3, w2, ln2_gain, ln2_bias):
    h = x
    for layer in range(DEPTH):
        mod = c @ w_ada[layer] + b_ada[layer]
        shift1, scale1, gate1, shift2, scale2, gate2 = [m[:, None, :] for m in jnp.split(mod, 6, axis=-1)]
        u = _layer_norm(h) * (1.0 + scale1) + shift1
        y = _token_mixing(u, w_in[layer], w_attn_out[layer], ret_decay_fwd[layer], ret_decay_bwd[layer],
                          ret_gn_gain[layer], w_ret_out[layer], w_out[layer])
        h = _layer_norm_affine(DEEPNORM_ALPHA * h + gate1 * y, ln1_gain[layer], ln1_bias[layer])
        u = _layer_norm(h) * (1.0 + scale2) + shift2
        y = _hierarchical_moe(u, w_coarse[layer], b_coarse[layer], w_fine[layer], b_fine[layer],
                              w1[layer], w3[layer], w2[layer])
        h = _layer_norm_affine(DEEPNORM_ALPHA * h + gate2 * y, ln2_gain[layer], ln2_bias[layer])
    return h
```

```python
import numpy as np
from contextlib import ExitStack
import concourse.bass as bass
import concourse.mybir as mybir
from concourse.bass_utils import run_bass_kernel_spmd

F32 = mybir.dt.float32
BF16 = mybir.dt.bfloat16
ALU = mybir.AluOpType
AF = mybir.ActivationFunctionType
AX = mybir.AxisListType

D = 1024
S_ALL = 4096
S_OWN = 2048
NT_OWN = 16
NT_ALL = 32
ALPHA = 2.0 ** 0.25
EPS = 1e-5
NEG = -30000.0
DEBUG_OUT = []


class Buf:
    __slots__ = ('name', 'w', 'r', 'sem', 'semval', 'kind')

    def __init__(self, name):
        self.name = name
        self.w = {}
        self.r = {}
        self.sem = None
        self.semval = 0


class Sched:
    ENG = ['pe', 'dve', 'act', 'pool', 'sp']

    def __init__(self, nc, stack):
        self.nc = nc
        self.stack = stack
        self.e = {}
        for name in self.ENG:
            sem = stack.enter_context(nc.semaphore(name + '_sem'))
            self.e[name] = dict(sem=sem, n=0, waited={}, prog=[], pending=[])
        self.bufs = []
        self.free = {'sw': [], 'hw': []}
        self.nsem = 0

    def buf(self, name):
        b = Buf(name)
        self.bufs.append(b)
        return b

    def _bufsem(self, b, eng='sp'):
        kind = 'sw' if eng == 'pool' else 'hw'
        if b.sem is not None:
            assert b.kind == kind, b.name
        if b.sem is None:
            b.kind = kind
            if self.free[kind]:
                b.sem, b.semval = self.free[kind].pop()
            else:
                b.sem = self.stack.enter_context(self.nc.semaphore('d%d' % self.nsem))
                b.semval = 0
                self.nsem += 1
        return b.sem

    def op(self, eng, fn, reads=(), writes=(), part=False, dma=False, sembuf=None):
        E = self.e[eng]
        deps = []
        for b in reads:
            for d in b.w.values():
                deps.append((d, 0))
        for b in writes:
            if not part:
                for d in b.w.values():
                    deps.append((d, 1))
            for d in b.r.values():
                deps.append((d, 2))
        waits = E['pending']
        E['pending'] = []
        for (sem, val, src, is_dma), kind in deps:
            if not is_dma and src == eng:
                if eng == 'pe' or kind == 2:
                    continue
            k = id(sem)
            if E['waited'].get(k, 0) >= val:
                continue
            E['waited'][k] = val
            waits.append((sem, val))
        if dma:
            dst = sembuf if sembuf is not None else writes[0]
            sem = self._bufsem(dst, eng)
            dst.semval += 16
            ev = (sem, dst.semval, eng, True)
            inc = 16
        else:
            E['n'] += 1
            sem = E['sem']
            ev = (sem, E['n'], eng, False)
            inc = 1
        k = id(sem)
        for b in writes:
            if not part:
                b.w = {}
                b.r = {}
            b.w[k] = ev
        for b in reads:
            b.r[k] = ev
        E['prog'].append((waits, fn, sem, inc))

    def barrier(self):
        evs = {}
        for b in self.bufs:
            for dd in (b.w, b.r):
                for k, d in dd.items():
                    if k not in evs or evs[k][1] < d[1]:
                        evs[k] = d
        for name in self.ENG:
            E = self.e[name]
            for k, (sem, val, src, is_dma) in evs.items():
                if E['waited'].get(k, 0) >= val:
                    continue
                if not is_dma and src == name:
                    continue
                E['waited'][k] = val
                E['pending'].append((sem, val))
        for b in self.bufs:
            if b.sem is not None:
                self.free[b.kind].append((b.sem, b.semval))
                b.sem = None

    def emit(self, block):
        sched = self

        def run(engname, eng):
            E = sched.e[engname]
            for waits, fn, sem, inc in E['prog']:
                for s, v in waits:
                    eng.wait_ge(s, v)
                ins = fn(eng)
                ins.then_inc(sem, inc)
            for s, v in E['pending']:
                eng.wait_ge(s, v)

        @block.tensor
        def _(eng):
            run('pe', eng)

        @block.vector
        def _(eng):
            run('dve', eng)

        @block.scalar
        def _(eng):
            run('act', eng)

        @block.gpsimd
        def _(eng):
            run('pool', eng)

        @block.sync
        def _(eng):
            run('sp', eng)


class Arena:
    def __init__(self, t, n):
        self.t = t
        self.n = n
        self.p = 0

    def alloc(self, shape):
        sz = 1
        for s in shape[1:]:
            sz *= s
        sz = (sz + 15) // 16 * 16
        a = self.p
        self.p += sz
        assert self.p <= self.n, ('arena overflow', self.p, self.n)
        ap = self.t[0:shape[0], a:a + int(np.prod(shape[1:]))]
        if len(shape) == 3:
            ap = ap.rearrange("p (a b) -> p a b", a=shape[1])
        return ap


def build_nc(stop_after=None, ne=32):
    nc = bass.Bass("TRN2", target_bir_lowering=False)

    def din(name, shape, dt=F32):
        return nc.dram_tensor(name, shape, dt, kind="ExternalInput").ap()

    def dscr(name, shape, dt):
        kind = "ExternalOutput" if name in DEBUG_OUT else "Internal"
        return nc.dram_tensor(name, shape, dt, kind=kind).ap()

    x_d = din("x", [S_ALL, D])
    cbc_d = din("cbc", [128, 8, 128])
    wada_d = din("w_ada", [D, 6 * D])
    bada_d = din("bada", [128, 6 * D])
    win_d = din("w_in", [D, 12800])
    wa_d = din("w_attn_out", [512, D])
    wr_d = din("w_ret_out", [2048, D])
    wo_d = din("w_out", [D, D])
    retlog_d = din("retlog", [128, 8])
    gng_d = din("gng", [128, 2048])
    ln1g_d = din("ln1g", [128, D])
    ln1b_d = din("ln1b", [128, D])
    ln2g_d = din("ln2g", [128, D])
    ln2b_d = din("ln2b", [128, D])
    wrt_d = din("wrt", [D, 36])
    brt_d = din("brt", [128, 36])
    w1_d = din("w1", [ne, D, 512])
    w3_d = din("w3", [ne, D, 512])
    w2_d = din("w2", [ne, 512, D])
    abias_d = din("abias", [128, 12 * 4 * 128])
    rtab_d = din("rtab", [128, 4 * 128 + 8])
    identf_d = din("identf", [128, 128])
    identb_d = din("identb", [128, 128], BF16)
    out_d = nc.dram_tensor("out", [S_OWN, D], F32, kind="ExternalOutput").ap()

    qTa_s = dscr("qTa_s", [12, 128, S_OWN], BF16)
    kTa_s = dscr("kTa_s", [12, 128, 3072], BF16)
    va_s = dscr("va_s", [3072, 1536], BF16)
    qTr_s = dscr("qTr_s", [8, 128, S_OWN], BF16)
    kTr_s = dscr("kTr_s", [8, 128, S_OWN], BF16)
    krF_s = dscr("krF_s", [S_OWN, 1024], BF16)
    krB_s = dscr("krB_s", [S_ALL, 1024], BF16)
    vr_s = dscr("vr_s", [S_ALL, 2048], BF16)
    srg_s = dscr("srg_s", [S_OWN, 2048], BF16)
    sga_s = dscr("sga_s", [S_OWN, 1024], BF16)
    sgr_s = dscr("sgr_s", [S_OWN, 1024], BF16)
    ao_s = dscr("ao_s", [S_OWN, 12, 130], F32)
    retg_s = dscr("retg_s", [S_OWN, 2048], BF16)
    h1_s = dscr("h1_s", [S_OWN, D], F32)

    with ExitStack() as st:
        S = Sched(nc, st)
        def finish():
            S.barrier()
            blk = st.enter_context(nc.Block())
            S.emit(blk)
            return nc
        NF, NB = 26624, 45056
        arF_t = st.enter_context(nc.sbuf_tensor("arF", [128, NF], F32))
        arB_t = st.enter_context(nc.sbuf_tensor("arB", [128, NB], BF16))
        arF = Arena(arF_t, NF)
        arB = Arena(arB_t, NB)
        PS = [st.enter_context(nc.psum_tensor("ps%d" % i, [128, 512], F32)) for i in range(8)]
        PB = [S.buf("ps%d" % i) for i in range(8)]
        psi = [0]

        def psn():
            i = psi[0] % 8
            psi[0] += 1
            return PS[i], PB[i]

        class Ring:
            def __init__(self, ar, name, shape, n):
                self.t = [ar.alloc(shape) for _ in range(n)]
                self.b = [S.buf(name + str(i)) for i in range(n)]
                self.i = 0
                self.n = n

            def next(self):
                k = self.i % self.n
                self.i += 1
                return self.t[k], self.b[k]

        def load(dst, src, b, part=False, cast=False, reads=()):
            q = 'pool' if cast else 'sp'
            S.op(q, lambda e: e.dma_start(out=dst, in_=src), reads=list(reads), writes=[b], part=part, dma=True)

        def store(dst, src, db, sb_, q='sp'):
            S.op(q, lambda e: e.dma_start(out=dst, in_=src), reads=[sb_], writes=[db], part=True, dma=True, sembuf=sb_)

        def TT(eng, out, a, b, op, reads, writes, part=False):
            S.op(eng, lambda e: e.tensor_tensor(out, a, b, op), reads=reads, writes=writes, part=part)

        def TS(eng, out, a, s1, s2, op0, op1, reads, writes, part=False):
            if op1 is None:
                S.op(eng, lambda e: e.tensor_scalar(out, a, s1, None, op0), reads=reads, writes=writes, part=part)
            else:
                S.op(eng, lambda e: e.tensor_scalar(out, a, s1, s2, op0, op1), reads=reads, writes=writes, part=part)

        def STT(out, a, s, b, op0, op1, reads, writes, part=False):
            S.op('dve', lambda e: e.scalar_tensor_tensor(out, a, s, b, op0, op1), reads=reads, writes=writes, part=part)

        def ACT(out, in_, func, reads, writes, bias=None, scale=None, accum=None, part=False):
            kw = {}
            if bias is not None:
                kw['bias'] = bias
            if scale is not None:
                kw['scale'] = scale
            if accum is not None:
                kw['accum_out'] = accum
            S.op('act', lambda e: e.activation(out, in_, func, **kw), reads=reads, writes=writes, part=part)

        def MM(out, lhsT, rhs, start, stop, reads, writes):
            S.op('pe', lambda e: e.matmul(out, lhsT, rhs, start=start, stop=stop), reads=reads, writes=writes,
                 part=not start)

        def TR(out, in_, ident, reads, writes, part):
            S.op('pe', lambda e: e.transpose(out, in_, ident), reads=reads, writes=writes, part=part)

        pF0 = arF.p
        identf = arF.alloc([128, 128]); b_identf = S.buf('identf')
        load(identf, identf_d[:, :], b_identf)
        epsc = arF.alloc([128, 16]); b_epsc = S.buf('epsc')
        S.op('dve', lambda e: e.memset(epsc[:, 0:1], EPS), writes=[b_epsc])
        kdt = arF.alloc([128, 16]); b_kdt = S.buf('kdt')
        lgt = arF.alloc([128, 16]); b_lgt = S.buf('lgt')
        rtab = arF.alloc([128, 4 * 128 + 8]); b_rtab = S.buf('rtab')
        load(rtab, rtab_d[:, :], b_rtab)
        gate2bc = arF.alloc([128, D]); b_g2 = S.buf('gate2bc')
        gate32 = arF.alloc([128, NT_OWN, 32]); b_gate = [S.buf('gate%d' % t) for t in range(NT_OWN)]
        pF_mod = arF.p
        modbc = arF.alloc([128, 6 * D]); b_mod = S.buf('modbc')
        pB0 = arB.p
        identb = arB.alloc([128, 128]); b_identb = S.buf('identb')
        load(identb, identb_d[:, :], b_identb)
        pF_base, pB_base = arF.p, arB.p

        def ln_stats(src, b_src, rstd_ring, small_ring):
            n = src.shape[1]
            sm, b_sm = small_ring.next()
            nst = n // 512
            for j in range(nst):
                S.op('dve', lambda e, j=j: e.bn_stats(sm[:, 6 * j:6 * j + 6], src[:, 512 * j:512 * (j + 1)]),
                     reads=[b_src], writes=[b_sm], part=(j > 0))
            S.op('dve', lambda e: e.bn_aggr(sm[:, 12:14], sm[:, 0:6 * nst]), reads=[b_sm], writes=[b_sm], part=True)
            ACT(sm[:, 14:15], sm[:, 13:14], AF.Sqrt, [b_sm, b_epsc], [b_sm], bias=epsc[:, 0:1], scale=1.0, part=True)
            S.op('dve', lambda e: e.reciprocal(sm[:, 15:16], sm[:, 14:15]), reads=[b_sm], writes=[b_sm], part=True)
            STT(sm[:, 16:17], sm[:, 12:13], -1.0, sm[:, 15:16], ALU.mult, ALU.mult, [b_sm], [b_sm], part=True)
            return sm[:, 15:16], sm[:, 16:17], b_sm

        def interleave(gens, width):
            active = []
            it = iter(gens)
            while True:
                if len(active) < width:
                    g_ = next(it, None)
                    if g_ is not None:
                        active.append(g_)
                if not active:
                    break
                for g_ in list(active):
                    try:
                        next(g_)
                    except StopIteration:
                        active.remove(g_)

        cbc = arF.alloc([128, 8, 128]); b_cbc = S.buf('cbc')
        load(cbc, cbc_d[:, :, :], b_cbc)
        wring = Ring(arF, 'wada', [128, 8, 512], 2)
        bring = Ring(arF, 'bada', [128, 512], 2)
        for blk in range(12):
            wt, b_wt = wring.next()
            bt, b_bt = bring.next()
            load(wt, wada_d[:, blk * 512:(blk + 1) * 512].rearrange("(kc k) n -> k kc n", k=128), b_wt)
            load(bt, bada_d[:, blk * 512:(blk + 1) * 512], b_bt)
            ps, b_ps = psn()
            for kc in range(8):
                MM(ps[:, :], cbc[:, kc, :], wt[:, kc, :], kc == 0, kc == 7, [b_cbc, b_wt], [b_ps])
            add1 = 1.0 if blk in (2, 3, 8, 9) else 0.0
            STT(modbc[:, blk * 512:(blk + 1) * 512], ps[:, :], add1, bt, ALU.add, ALU.add, [b_ps, b_bt], [b_mod],
                part=(blk > 0))
        retlog = arF.alloc([128, 8]); b_retlog = S.buf('retlog')
        load(retlog, retlog_d[:, :], b_retlog)
        ACT(lgt[:, 8:16], retlog, AF.Exp, [b_retlog], [b_lgt], scale=-1.0)
        ACT(lgt[:, 8:16], lgt[:, 8:16], AF.Ln, [b_lgt], [b_lgt], bias=1.0, scale=1.0, part=True)
        TS('dve', lgt[:, 0:8], lgt[:, 8:16], -1.0, None, ALU.mult, None, [b_lgt], [b_lgt], part=True)
        for h in range(4):
            ACT(kdt[:, h:h + 1], rtab[:, 512:513], AF.Exp, [b_rtab, b_lgt], [b_kdt], scale=lgt[:, h:h + 1], part=(h > 0))
            ACT(kdt[:, 4 + h:5 + h], rtab[:, 513:514], AF.Exp, [b_rtab, b_lgt], [b_kdt], scale=lgt[:, 4 + h:5 + h], part=True)
            ACT(kdt[:, 8 + h:9 + h], rtab[:, 514:515], AF.Exp, [b_rtab, b_lgt], [b_kdt], scale=lgt[:, h:h + 1], part=True)
            ACT(kdt[:, 12 + h:13 + h], rtab[:, 514:515], AF.Exp, [b_rtab, b_lgt], [b_kdt], scale=lgt[:, 4 + h:5 + h], part=True)
        TS('dve', kdt[:, 0:8], kdt[:, 0:8], 0.0625, None, ALU.mult, None, [b_kdt], [b_kdt], part=True)
        S.barrier()
        arF.p, arB.p = pF_base, pB_base

        if stop_after == 'A':
            return finish()
        uT = arB.alloc([128, 8, S_ALL])
        b_uT = [S.buf('uT%d' % t) for t in range(NT_ALL)]
        pB_c = arB.p
        xring = Ring(arF, 'x', [128, D], 5)
        xnring = Ring(arF, 'xn', [128, D], 4)
        t1ring = Ring(arF, 't1', [128, D], 4)
        smring = Ring(arF, 'sm', [128, 32], 8)
        ubring = Ring(arB, 'ub', [128, D], 4)

        def phB_tile(t):
            xs, b_xs = xring.next()
            load(xs, x_d[t * 128:(t + 1) * 128, :], b_xs)
            yield
            rstd, nmr, b_sm = ln_stats(xs, b_xs, None, smring)
            yield
            xn, b_xn = xnring.next()
            ACT(xn, xs, AF.Identity, [b_xs, b_sm], [b_xn], bias=nmr, scale=rstd)
            yield
            t1, b_t1 = t1ring.next()
            TT('dve', t1, xn, modbc[:, 1024:2048], ALU.mult, [b_xn, b_mod], [b_t1])
            yield
            ub, b_ub = ubring.next()
            TT('pool', ub, t1, modbc[:, 0:1024], ALU.add, [b_t1, b_mod], [b_ub])
            yield
            ps, b_ps = psn()
            psb = ps[:, :].bitcast(BF16)
            for kc in range(8):
                TR(psb[:, kc * 128:(kc + 1) * 128], ub[:, kc * 128:(kc + 1) * 128], identb, [b_ub, b_identb], [b_ps],
                   part=(kc > 0))
            yield
            ACT(uT[:, :, t * 128:(t + 1) * 128], psb.rearrange("p (a b) -> p a b", a=8), AF.Copy, [b_ps], [b_uT[t]])
        interleave((phB_tile(t) for t in range(NT_ALL)), 4)
        S.barrier()
        arF.p, arB.p = pF_base, pB_c

        if stop_after == 'B':
            return finish()
        wring = Ring(arB, 'win', [128, 8, 512], 2)
        stg = Ring(arB, 'stg', [128, 512], 6)
        sb_q = S.buf('qTa_s'); sb_k = S.buf('kTa_s'); sb_v = S.buf('va_s')
        sb_qr = S.buf('qTr_s'); sb_kr = S.buf('kTr_s'); sb_krF = S.buf('krF_s'); sb_krB = S.buf('krB_s')
        sb_vr = S.buf('vr_s'); sb_srg = S.buf('srg_s'); sb_sga = S.buf('sga_s'); sb_sgr = S.buf('sgr_s')
        evc = [0]

        def evac_copy(dst, src, reads, writes, scale=None):
            evc[0] += 1
            if evc[0] % 2 == 0:
                ACT(dst, src, AF.Copy, reads, writes, scale=scale)
            elif scale is not None:
                TS('dve', dst, src, scale, None, ALU.mult, None, reads, writes)
            else:
                S.op('dve', lambda e: e.tensor_copy(dst, src), reads=reads, writes=writes)

        blocks = []
        for i in range(3):
            blocks.append((i * 512, 'aq'))
        for i in range(3):
            blocks.append((1536 + i * 512, 'ak'))
        for i in range(3):
            blocks.append((3072 + i * 512, 'av'))
        for i in range(2):
            blocks.append((4608 + i * 512, 'rq'))
        for i in range(2):
            blocks.append((5632 + i * 512, 'rk'))
        for i in range(4):
            blocks.append((6656 + i * 512, 'rv'))
        for i in range(4):
            blocks.append((8704 + i * 512, 'rg'))
        for i in range(2):
            blocks.append((10752 + i * 512, 'ga'))
        for i in range(2):
            blocks.append((11776 + i * 512, 'gr'))
        import os as _os
        if _os.environ.get('C_KINDS'):
            blocks = [b for b in blocks if b[1] in _os.environ['C_KINDS'].split(',')]
        wt_list = []

        def issue_w(j):
            c0, _ = blocks[j]
            wt, b_wt = wring.next()
            load(wt, win_d[:, c0:c0 + 512].rearrange("(kc k) n -> k kc n", k=128), b_wt, cast=True)
            wt_list.append((wt, b_wt))

        def fm(wt, b_wt, j, ntb, dst_fn, b_dst, scale=None):
            for tb in range(ntb):
                ps, b_ps = psn()
                for kc in range(8):
                    MM(ps[:, :], wt[:, kc, j * 128:(j + 1) * 128], uT[:, kc, tb * 512:(tb + 1) * 512], kc == 0, kc == 7,
                       [b_wt] + b_uT[tb * 4:tb * 4 + 4], [b_ps])
                sg, b_sg = stg.next()
                evac_copy(sg, ps[:, :], [b_ps], [b_sg], scale=scale)
                store(dst_fn(tb), sg, b_dst, b_sg)

        def tm(wt, b_wt, tiles, evac_fn):
            for t in tiles:
                ps, b_ps = psn()
                for kc in range(8):
                    MM(ps[:, :], uT[:, kc, t * 128:(t + 1) * 128], wt[:, kc, :], kc == 0, kc == 7, [b_wt, b_uT[t]], [b_ps])
                evac_fn(t, ps, b_ps)

        issue_w(0)
        for bi, (c0, kind) in enumerate(blocks):
            if bi + 1 < len(blocks):
                issue_w(bi + 1)
            wt, b_wt = wt_list[bi]
            if kind == 'aq':
                for j in range(4):
                    ch = (c0 // 128) + j
                    fm(wt, b_wt, j, 4, lambda tb, ch=ch: qTa_s[ch, :, tb * 512:(tb + 1) * 512], sb_q)
            elif kind == 'ak':
                for j in range(4):
                    ch = ((c0 - 1536) // 128) + j
                    fm(wt, b_wt, j, (5, 5, 6)[(c0 - 1536) // 512], lambda tb, ch=ch: kTa_s[ch, :, tb * 512:(tb + 1) * 512], sb_k)
            elif kind == 'rq':
                for j in range(4):
                    ch = ((c0 - 4608) // 128) + j
                    fm(wt, b_wt, j, 4, lambda tb, ch=ch: qTr_s[ch, :, tb * 512:(tb + 1) * 512], sb_qr)
            elif kind == 'av':
                cc = c0 - 3072

                def ev(t, ps, b_ps, cc=cc):
                    sg, b_sg = stg.next()
                    evac_copy(sg, ps[:, :], [b_ps], [b_sg])
                    store(va_s[t * 128:(t + 1) * 128, cc:cc + 512], sg, sb_v, b_sg)
                tm(wt, b_wt, range((17, 18, 24)[cc // 512]), ev)
            elif kind == 'rk':
                cc = c0 - 5632
                rkm = _os.environ.get('RK_MODE', 'fm,tmB,tmF')
                for j in range(4 if 'fm' in rkm else 0):
                    ch = (cc // 128) + j
                    fm(wt, b_wt, j, 4, lambda tb, ch=ch: kTr_s[ch, :, tb * 512:(tb + 1) * 512], sb_kr, scale=0.0625)

                def ev(t, ps, b_ps, cc=cc):
                    h0 = cc // 256
                    sg, b_sg = stg.next()
                    for hh in range(2 if 'tmB' in rkm else 0):
                        TS('dve', sg[:, hh * 256:(hh + 1) * 256], ps[:, hh * 256:(hh + 1) * 256],
                           kdt[:, 4 + h0 + hh:5 + h0 + hh], None, ALU.mult, None, [b_ps, b_kdt], [b_sg], part=(hh > 0))
                    store(krB_s[t * 128:(t + 1) * 128, cc:cc + 512], sg, sb_krB, b_sg)
                    if t < NT_OWN and 'tmF' in rkm:
                        sg2, b_sg2 = stg.next()
                        for hh in range(2):
                            TS('dve', sg2[:, hh * 256:(hh + 1) * 256], ps[:, hh * 256:(hh + 1) * 256],
                               kdt[:, h0 + hh:h0 + hh + 1], None, ALU.mult, None, [b_ps, b_kdt], [b_sg2], part=(hh > 0))
                        store(krF_s[t * 128:(t + 1) * 128, cc:cc + 512], sg2, sb_krF, b_sg2)
                tm(wt, b_wt, range(NT_ALL), ev)
            elif kind == 'rv':
                cc = c0 - 6656

                def ev(t, ps, b_ps, cc=cc):
                    sg, b_sg = stg.next()
                    evac_copy(sg, ps[:, :], [b_ps], [b_sg])
                    store(vr_s[t * 128:(t + 1) * 128, cc:cc + 512], sg, sb_vr, b_sg)
                tm(wt, b_wt, range(NT_ALL), ev)
            else:
                base, dst, b_dst, func = {'rg': (8704, srg_s, sb_srg, AF.Silu), 'ga': (10752, sga_s, sb_sga, AF.Sigmoid),
                                          'gr': (11776, sgr_s, sb_sgr, AF.Sigmoid)}[kind]
                cc = c0 - base

                def ev(t, ps, b_ps, cc=cc, dst=dst, b_dst=b_dst, func=func):
                    sg, b_sg = stg.next()
                    ACT(sg, ps[:, :], func, [b_ps], [b_sg])
                    store(dst[t * 128:(t + 1) * 128, cc:cc + 512], sg, b_dst, b_sg)
                tm(wt, b_wt, range(NT_OWN), ev)
        S.barrier()
        arF.p, arB.p = pF_base, pB0 + 128
        pB_base2 = arB.p

        if stop_after == 'C':
            return finish()
        abias = arF.alloc([128, 12 * 4 * 128]); b_abias = S.buf('abias')
        load(abias, abias_d[:, :], b_abias)
        qring = Ring(arB, 'qh', [128, S_OWN], 2)
        kring = Ring(arB, 'kh', [128, 3072], 2)
        vring = Ring(arB, 'vt', [128, 128], 20)
        pring = Ring(arB, 'P', [128, 256], 8)
        ptring = Ring(arB, 'PT', [128, 256], 8)
        sbring = Ring(arF, 'Sb', [128, 256], 8)
        osring = Ring(arF, 'os', [128, 130], 10)
        sb_ao = S.buf('ao_s')

        def attn_item(h, d, r, i, qv, kv, aov, vav, b_qh, b_kh):
            n0 = 128 * i
            vts = []
            for j in (i, i + 1):
                vt, b_vt = vring.next()
                ks = 0 if j == 0 else 64 + 128 * (j - 1)
                load(vt, vav[r, ks:ks + 128, h * 128:(h + 1) * 128], b_vt, reads=[sb_v])
                vts.append((vt, b_vt))
            qa = qv[:, r, n0:n0 + 128]
            ps, b_ps = psn()
            for jj, j in enumerate((i, i + 1)):
                ks = 0 if j == 0 else 64 + 128 * (j - 1)
                MM(ps[:, jj * 128:(jj + 1) * 128], qa, kv[:, r, ks:ks + 128], True, True, [b_qh, b_kh], [b_ps])
            yield
            sbt, b_sbt = sbring.next()
            bo = (h * 4 + (2 if i == 0 else 0)) * 128
            STT(sbt, ps[:, 0:256], 128.0 ** -0.5, abias[:, bo:bo + 256], ALU.mult, ALU.add, [b_ps, b_abias], [b_sbt])
            ost, b_ost = osring.next()
            S.op('dve', lambda e: e.tensor_reduce(ost[:, 128:129], sbt, AX.X, ALU.max, negate=True),
                 reads=[b_sbt], writes=[b_ost])
            yield
            pt_, b_p = pring.next()
            ACT(pt_, sbt, AF.Exp, [b_sbt, b_ost], [b_p, b_ost], bias=ost[:, 128:129], scale=1.0,
                accum=ost[:, 129:130], part=True)
            yield
            ps2, b_ps2 = psn()
            ps2b = ps2[:, :].bitcast(BF16)
            for jj in range(2):
                TR(ps2b[:, jj * 128:(jj + 1) * 128], pt_[:, jj * 128:(jj + 1) * 128], identb, [b_p, b_identb],
                   [b_ps2], part=(jj > 0))
            yield
            ptt, b_ptt = ptring.next()
            S.op('dve', lambda e: e.tensor_copy(ptt, ps2b[:, 0:256]), reads=[b_ps2], writes=[b_ptt])
            yield
            ps3, b_ps3 = psn()
            for jj in range(2):
                vt, b_vt = vts[jj]
                MM(ps3[:, 0:128], ptt[:, jj * 128:(jj + 1) * 128], vt, jj == 0, jj == 1, [b_ptt, b_vt], [b_ps3])
            yield
            ACT(ost[:, 0:128], ps3[:, 0:128], AF.Copy, [b_ps3], [b_ost], part=True)
            store(aov[r, n0:n0 + 128, h, :], ost, sb_ao, b_ost, q='act')

        def attn_items():
            for h in range(12):
                d = (1, 4, 16)[h // 4]
                nq = (S_OWN // d) // 128
                qh, b_qh = qring.next()
                kh, b_kh = kring.next()
                load(qh, qTa_s[h, :, :], b_qh, reads=[sb_q])
                kw_ = (2560, 2560, 3072)[h // 4]
                load(kh[:, 0:kw_], kTa_s[h, :, 0:kw_], b_kh, reads=[sb_k])
                qv = qh.rearrange("p (n d) -> p d n", d=d)
                kv = kh.rearrange("p (n d) -> p d n", d=d)
                aov = ao_s.rearrange("(n d) h c -> d n h c", d=d)
                vav = va_s.rearrange("(n d) c -> d n c", d=d)
                for r in range(d):
                    for i in range(nq):
                        yield attn_item(h, d, r, i, qv, kv, aov, vav, b_qh, b_kh)
        interleave(attn_items(), 8)
        S.barrier()
        arF.p, arB.p = pF_base, pB_base2

        if stop_after == 'D':
            return finish()
        gng = arF.alloc([128, 2048]); b_gng = S.buf('gng')
        load(gng, gng_d[:, :], b_gng)
        NSL = 2
        hs = []
        for sl in range(NSL):
            hs.append(dict(
                DT=arF.alloc([128, 128]), b_DT=S.buf('DT%d' % sl),
                qdt=arF.alloc([128, 4, 128]), b_qdt=S.buf('qdt%d' % sl),
                tmpd=arF.alloc([128, 128]), b_tmpd=S.buf('tmpd%d' % sl),
                Sf=arF.alloc([128, 2, 512]), b_Sf=S.buf('Sf%d' % sl),
                Sb=arF.alloc([128, 2, 512]), b_Sb=S.buf('Sb%d' % sl),
                Sfb=arB.alloc([128, 2, 512]), b_Sfb=S.buf('Sfb%d' % sl),
                Sbb=arB.alloc([128, 2, 512]), b_Sbb=S.buf('Sbb%d' % sl),
                yb=arB.alloc([128, 16, 512]), b_yb=[S.buf('yb%d_%d' % (sl, i)) for i in range(16)],
                qT=arB.alloc([128, 2, S_OWN]), b_qT=S.buf('qTr%d' % sl),
                kT=arB.alloc([128, 2, S_OWN]), b_kT=S.buf('kTr%d' % sl),
            ))
        ktile = Ring(arB, 'ktile', [128, 256], 5)
        vtile = Ring(arB, 'vtile', [128, 512], 5)
        srgt = Ring(arB, 'srgt', [128, 512], 3)
        qsc = Ring(arB, 'qsc', [128, 2, 128], 4)
        innr = Ring(arB, 'innT', [128, 128], 3)
        rgst = Ring(arB, 'rgst', [128, 512], 2)
        ysbr = Ring(arF, 'ysb', [128, 512], 3)
        ynr = Ring(arF, 'yn', [128, 512], 3)
        ytr = Ring(arF, 'yt', [128, 512], 3)
        smring = Ring(arF, 'smE', [128, 32], 6)
        sb_retg = S.buf('retg_s')

        def ret_head(h, H):
            DT, b_DT, qdt, b_qdt, tmpd, b_tmpd = H['DT'], H['b_DT'], H['qdt'], H['b_qdt'], H['tmpd'], H['b_tmpd']
            Sf, b_Sf, Sb_, b_Sb, Sfb, b_Sfb, Sbb, b_Sbb = H['Sf'], H['b_Sf'], H['Sb'], H['b_Sb'], H['Sfb'], H['b_Sfb'], H['Sbb'], H['b_Sbb']
            yb, b_yb, qT, b_qT, kT, b_kT = H['yb'], H['b_yb'], H['qT'], H['b_qT'], H['kT'], H['b_kT']
            lgF = lgt[:, h:h + 1]
            lgB = lgt[:, 4 + h:5 + h]
            TS('dve', tmpd, rtab[:, 0:128], lgF, None, ALU.mult, None, [b_rtab, b_lgt], [b_tmpd])
            STT(tmpd, rtab[:, 128:256], lgB, tmpd, ALU.mult, ALU.add, [b_rtab, b_lgt, b_tmpd], [b_tmpd], part=True)
            ACT(DT, tmpd, AF.Exp, [b_tmpd], [b_DT])
            for c in range(2):
                ACT(qdt[:, c, :], rtab[:, 256:384], AF.Exp, [b_rtab, b_lgt], [b_qdt], scale=lgF, part=(c > 0))
                ACT(qdt[:, 2 + c, :], rtab[:, 384:512], AF.Exp, [b_rtab, b_lgt], [b_qdt], scale=lgB, part=True)
            for c in range(2):
                load(qT[:, c, :], qTr_s[2 * h + c, :, :], b_qT, part=(c > 0), reads=[sb_qr])
                load(kT[:, c, :], kTr_s[2 * h + c, :, :], b_kT, part=(c > 0), reads=[sb_kr])
            yield
            for i in range(NT_ALL - 1, -1, -1):
                kt, b_kt = ktile.next()
                vt, b_vt = vtile.next()
                load(kt, krB_s[i * 128:(i + 1) * 128, h * 256:(h + 1) * 256], b_kt, reads=[sb_krB])
                load(vt, vr_s[i * 128:(i + 1) * 128, h * 512:(h + 1) * 512], b_vt, reads=[sb_vr])
                if i < NT_OWN:
                    qb, b_qb = qsc.next()
                    TT('dve', qb, qT[:, :, i * 128:(i + 1) * 128], qdt[:, 2:4, :], ALU.mult, [b_qT, b_qdt], [b_qb])
                    ps, b_ps = psn()
                    for c in range(2):
                        MM(ps[:, :], qb[:, c, :], Sbb[:, c, :], c == 0, c == 1, [b_qb, b_Sbb], [b_ps])
                    ACT(yb[:, i, :], ps[:, :], AF.Copy, [b_ps], [b_yb[i]])
                if i > 0:
                    pss = []
                    for c in range(2):
                        ps, b_ps = psn()
                        MM(ps[:, :], kt[:, c * 128:(c + 1) * 128], vt, True, True, [b_kt, b_vt], [b_ps])
                        pss.append((ps, b_ps))
                    yield
                    for c in range(2):
                        ps, b_ps = pss[c]
                        if i == NT_ALL - 1:
                            S.op('dve', lambda e, c=c, ps=ps: e.tensor_copy(Sb_[:, c, :], ps[:, :]), reads=[b_ps],
                                 writes=[b_Sb], part=(c > 0))
                        else:
                            STT(Sb_[:, c, :], Sb_[:, c, :], kdt[:, 12 + h:13 + h], ps[:, :], ALU.mult, ALU.add,
                                [b_Sb, b_ps, b_kdt], [b_Sb], part=True)
                    yield
                    if i <= NT_OWN:
                        ACT(Sbb, Sb_, AF.Copy, [b_Sb], [b_Sbb])
                    yield
            for i in range(NT_OWN):
                kt, b_kt = ktile.next()
                vt, b_vt = vtile.next()
                sr, b_sr = srgt.next()
                load(kt, krF_s[i * 128:(i + 1) * 128, h * 256:(h + 1) * 256], b_kt, reads=[sb_krF])
                load(vt, vr_s[i * 128:(i + 1) * 128, h * 512:(h + 1) * 512], b_vt, reads=[sb_vr])
                load(sr, srg_s[i * 128:(i + 1) * 128, h * 512:(h + 1) * 512], b_sr, reads=[sb_srg])
                ps, b_ps = psn()
                for c in range(2):
                    MM(ps[:, 0:128], kT[:, c, i * 128:(i + 1) * 128], qT[:, c, i * 128:(i + 1) * 128], c == 0, c == 1,
                       [b_kT, b_qT], [b_ps])
                pss = []
                if i < NT_OWN - 1:
                    for c in range(2):
                        psu, b_psu = psn()
                        MM(psu[:, :], kt[:, c * 128:(c + 1) * 128], vt, True, True, [b_kt, b_vt], [b_psu])
                        pss.append((psu, b_psu))
                yield
                inn, b_inn = innr.next()
                TT('dve', inn, ps[:, 0:128], DT, ALU.mult, [b_ps, b_DT], [b_inn])
                if i > 0:
                    qf, b_qf = qsc.next()
                    TT('dve', qf, qT[:, :, i * 128:(i + 1) * 128], qdt[:, 0:2, :], ALU.mult, [b_qT, b_qdt], [b_qf])
                yield
                psy, b_psy = psn()
                MM(psy[:, :], inn, vt, True, i == 0, [b_inn, b_vt], [b_psy])
                if i > 0:
                    for c in range(2):
                        MM(psy[:, :], qf[:, c, :], Sfb[:, c, :], False, c == 1, [b_qf, b_Sfb], [b_psy])
                yield
                ysb, b_ysb = ysbr.next()
                TT('dve', ysb, psy[:, :], yb[:, i, :], ALU.add, [b_psy, b_yb[i]], [b_ysb])
                if i < NT_OWN - 1:
                    for c in range(2):
                        psu, b_psu = pss[c]
                        if i == 0:
                            S.op('dve', lambda e, c=c, psu=psu: e.tensor_copy(Sf[:, c, :], psu[:, :]), reads=[b_psu],
                                 writes=[b_Sf], part=(c > 0))
                        else:
                            STT(Sf[:, c, :], Sf[:, c, :], kdt[:, 8 + h:9 + h], psu[:, :], ALU.mult, ALU.add,
                                [b_Sf, b_psu, b_kdt], [b_Sf], part=True)
                    yield
                    ACT(Sfb, Sf, AF.Copy, [b_Sf], [b_Sfb])
                yield
                rstd, nmr, b_sm = ln_stats(ysb, b_ysb, None, smring)
                yield
                yn, b_yn = ynr.next()
                ACT(yn, ysb, AF.Identity, [b_ysb, b_sm], [b_yn], bias=nmr, scale=rstd)
                yield
                yt, b_yt = ytr.next()
                TT('dve', yt, yn, gng[:, h * 512:(h + 1) * 512], ALU.mult, [b_yn, b_gng], [b_yt])
                rg, b_rg = rgst.next()
                TT('pool', rg, yt, sr, ALU.mult, [b_yt, b_sr], [b_rg])
                store(retg_s[i * 128:(i + 1) * 128, h * 512:(h + 1) * 512], rg, sb_retg, b_rg, q='pool')
                yield

        interleave((ret_head(h, hs[h % NSL]) for h in range(4)), NSL)
        S.barrier()
        arF.p, arB.p = pF_base, pB_base2

        if stop_after == 'E':
            return finish()
        Wa = arB.alloc([128, 4, D]); b_Wa = S.buf('Wa')
        Wr = arB.alloc([128, 16, D]); b_Wr = S.buf('Wr')
        Wo = arB.alloc([128, 8, D]); b_Wo = S.buf('Wo')
        load(Wa, wa_d.rearrange("(kc k) n -> k kc n", k=128), b_Wa, cast=True)
        for q4 in range(4):
            load(Wr[:, q4 * 4:(q4 + 1) * 4, :], wr_d[q4 * 512:(q4 + 1) * 512, :].rearrange("(kc k) n -> k kc n", k=128), b_Wr,
                 cast=True, part=(q4 > 0))
        for q4 in range(2):
            load(Wo[:, q4 * 4:(q4 + 1) * 4, :], wo_d[q4 * 512:(q4 + 1) * 512, :].rearrange("(kc k) n -> k kc n", k=128), b_Wo,
                 cast=True, part=(q4 > 0))
        ln1g = arF.alloc([128, D]); b_ln1g = S.buf('ln1g')
        ln1b = arF.alloc([128, D]); b_ln1b = S.buf('ln1b')
        load(ln1g, ln1g_d[:, :], b_ln1g)
        load(ln1b, ln1b_d[:, :], b_ln1b)
        aor = Ring(arF, 'ao', [128, 12, 130], 2)
        xr = Ring(arF, 'xF', [128, D], 2)
        t1r = Ring(arF, 't1F', [128, 512], 3)
        t2r = Ring(arF, 't2F', [128, 512], 3)
        tgr = Ring(arF, 'tgF', [128, D], 2)
        hpr = Ring(arF, 'hpF', [128, D], 2)
        hnr = Ring(arF, 'hnF', [128, D], 1)
        h1r = Ring(arF, 'h1F', [128, D], 2)
        smring = Ring(arF, 'smF', [128, 64], 6)
        rgr = Ring(arB, 'rgF', [128, 2048], 2)
        sgar = Ring(arB, 'sgaF', [128, D], 2)
        sgrr = Ring(arB, 'sgrF', [128, D], 2)
        atr = Ring(arB, 'atF', [128, 512], 2)
        atTr = Ring(arB, 'atTF', [128, 4, 128], 2)
        rgTr = Ring(arB, 'rgTF', [128, 16, 128], 1)
        mgr = Ring(arB, 'mgF', [128, D], 2)
        mgTr = Ring(arB, 'mgTF', [128, 8, 128], 1)
        sb_h1 = S.buf('h1_s')
        def f1_tile(t):
            rows = slice(t * 128, (t + 1) * 128)
            ao, b_ao = aor.next()
            load(ao, ao_s[rows, :, :], b_ao, reads=[sb_ao])
            rgt, b_rgt = rgr.next()
            load(rgt, retg_s[rows, :], b_rgt, reads=[sb_retg])
            sga, b_sga = sgar.next()
            load(sga, sga_s[rows, :], b_sga, reads=[sb_sga])
            sgr, b_sgr = sgrr.next()
            load(sgr, sgr_s[rows, :], b_sgr, reads=[sb_sgr])
            xs, b_xs = xr.next()
            load(xs, x_d[rows, :], b_xs)
            yield
            sm, b_sm = smring.next()
            nm = ao[:, :, 128]
            rs = ao[:, :, 129]
            TT('dve', sm[:, 0:4], nm[:, 0:4], nm[:, 4:8], ALU.min, [b_ao], [b_sm])
            TT('dve', sm[:, 0:4], sm[:, 0:4], nm[:, 8:12], ALU.min, [b_ao, b_sm], [b_sm], part=True)
            for g in range(3):
                TT('dve', sm[:, 4 + 4 * g:8 + 4 * g], nm[:, 4 * g:4 * g + 4], sm[:, 0:4], ALU.subtract, [b_ao, b_sm], [b_sm],
                   part=True)
            ACT(sm[:, 16:28], sm[:, 4:16], AF.Exp, [b_sm], [b_sm], scale=-1.0, part=True)
            TT('dve', sm[:, 28:40], sm[:, 16:28], rs, ALU.mult, [b_sm, b_ao], [b_sm], part=True)
            TT('dve', sm[:, 40:44], sm[:, 28:32], sm[:, 32:36], ALU.add, [b_sm], [b_sm], part=True)
            TT('dve', sm[:, 40:44], sm[:, 40:44], sm[:, 36:40], ALU.add, [b_sm], [b_sm], part=True)
            S.op('dve', lambda e, sm=sm: e.reciprocal(sm[:, 44:48], sm[:, 40:44]), reads=[b_sm], writes=[b_sm], part=True)
            for g in range(3):
                TT('dve', sm[:, 48 + 4 * g:52 + 4 * g], sm[:, 16 + 4 * g:20 + 4 * g], sm[:, 44:48], ALU.mult, [b_sm], [b_sm],
                   part=True)
            yield
            at, b_at = atr.next()
            t1, b_t1 = t1r.next()
            for h4 in range(4):
                TS('dve', t1[:, h4 * 128:(h4 + 1) * 128], ao[:, h4, 0:128], sm[:, 48 + h4:49 + h4], None, ALU.mult, None,
                   [b_ao, b_sm], [b_t1], part=(h4 > 0))
                STT(t1[:, h4 * 128:(h4 + 1) * 128], ao[:, 4 + h4, 0:128], sm[:, 52 + h4:53 + h4],
                    t1[:, h4 * 128:(h4 + 1) * 128], ALU.mult, ALU.add, [b_ao, b_sm, b_t1], [b_t1], part=True)
                STT(at[:, h4 * 128:(h4 + 1) * 128], ao[:, 8 + h4, 0:128], sm[:, 56 + h4:57 + h4],
                    t1[:, h4 * 128:(h4 + 1) * 128], ALU.mult, ALU.add, [b_ao, b_sm, b_t1], [b_at], part=(h4 > 0))
            yield
            ps, b_ps = psn()
            psb = ps[:, :].bitcast(BF16)
            for kc in range(4):
                TR(psb[:, kc * 128:(kc + 1) * 128], at[:, kc * 128:(kc + 1) * 128], identb, [b_at, b_identb], [b_ps], part=(kc > 0))
            atT, b_atT = atTr.next()
            S.op('dve', lambda e, atT=atT, psb=psb: e.tensor_copy(atT, psb[:, 0:512].rearrange("p (a b) -> p a b", a=4)),
                 reads=[b_ps], writes=[b_atT])
            yield
            rgT, b_rgT = rgTr.next()
            for half in range(2):
                ps, b_ps = psn()
                psb = ps[:, :].bitcast(BF16)
                for kc in range(8):
                    TR(psb[:, kc * 128:(kc + 1) * 128], rgt[:, (half * 8 + kc) * 128:(half * 8 + kc + 1) * 128], identb,
                       [b_rgt, b_identb], [b_ps], part=(kc > 0))
                ACT(rgT[:, half * 8:(half + 1) * 8, :], psb.rearrange("p (a b) -> p a b", a=8), AF.Copy, [b_ps], [b_rgT],
                    part=(half > 0))
            mg, b_mg = mgr.next()
            for half in range(2):
                cs = slice(half * 512, (half + 1) * 512)
                psa, b_psa = psn()
                for kc in range(4):
                    MM(psa[:, :], atT[:, kc, :], Wa[:, kc, cs], kc == 0, kc == 3, [b_atT, b_Wa], [b_psa])
                psr, b_psr = psn()
                for kc in range(16):
                    MM(psr[:, :], rgT[:, kc, :], Wr[:, kc, cs], kc == 0, kc == 15, [b_rgT, b_Wr], [b_psr])
                t1, b_t1 = t1r.next()
                t2, b_t2 = t2r.next()
                TT('dve', t1, psa[:, :], sga[:, cs], ALU.mult, [b_psa, b_sga], [b_t1])
                TT('dve', t2, psr[:, :], sgr[:, cs], ALU.mult, [b_psr, b_sgr], [b_t2])
                TT('pool', mg[:, cs], t1, t2, ALU.add, [b_t1, b_t2], [b_mg], part=(half > 0))
            yield
            ps, b_ps = psn()
            psb = ps[:, :].bitcast(BF16)
            for kc in range(8):
                TR(psb[:, kc * 128:(kc + 1) * 128], mg[:, kc * 128:(kc + 1) * 128], identb, [b_mg, b_identb], [b_ps], part=(kc > 0))
            yield
            mgT, b_mgT = mgTr.next()
            ACT(mgT, psb.rearrange("p (a b) -> p a b", a=8), AF.Copy, [b_ps], [b_mgT])
            tg, b_tg = tgr.next()
            for half in range(2):
                cs = slice(half * 512, (half + 1) * 512)
                psy, b_psy = psn()
                for kc in range(8):
                    MM(psy[:, :], mgT[:, kc, :], Wo[:, kc, cs], kc == 0, kc == 7, [b_mgT, b_Wo], [b_psy])
                TT('dve', tg[:, cs], psy[:, :], modbc[:, 2048 + half * 512:2048 + (half + 1) * 512], ALU.mult, [b_psy, b_mod],
                   [b_tg], part=(half > 0))
            yield
            hp, b_hp = hpr.next()
            STT(hp, xs, ALPHA, tg, ALU.mult, ALU.add, [b_xs, b_tg], [b_hp])
            yield
            rstd, nmr, b_sm2 = ln_stats(hp, b_hp, None, smring)
            yield
            hn, b_hn = hnr.next()
            ACT(hn, hp, AF.Identity, [b_hp, b_sm2], [b_hn], bias=nmr, scale=rstd)
            TT('dve', hn, hn, ln1g, ALU.mult, [b_hn, b_ln1g], [b_hn])
            h1, b_h1 = h1r.next()
            TT('pool', h1, hn, ln1b, ALU.add, [b_hn, b_ln1b], [b_h1])
            store(h1_s[rows, :], h1, sb_h1, b_h1, q='pool')
        interleave((f1_tile(t) for t in range(NT_OWN)), 2)
        S.barrier()
        arF.p, arB.p = pF_base, pB_base2

        if stop_after == 'F1':
            return finish()
        u2T = arB.alloc([128, 8, S_OWN]); b_u2T = [S.buf('u2T%d' % t) for t in range(NT_OWN)]
        pB_g = arB.p
        wrt = arF.alloc([128, 8, 36]); b_wrt = S.buf('wrt')
        load(wrt, wrt_d.rearrange("(kc k) n -> k kc n", k=128), b_wrt)
        brt = arF.alloc([128, 36]); b_brt = S.buf('brt')
        load(brt, brt_d[:, :], b_brt)
        h1r = Ring(arF, 'h1G', [128, D], 3)
        hnr = Ring(arF, 'hnG', [128, D], 3)
        tr_ = Ring(arF, 'tG', [128, D], 3)
        u2r = Ring(arF, 'u2G', [128, D], 3)
        u2t32 = Ring(arF, 'u2T32', [128, 8, 128], 3)
        smring = Ring(arF, 'smG', [128, 32], 6)
        rtr = Ring(arF, 'rt', [128, 96], 5)
        u2br = Ring(arB, 'u2b', [128, D], 3)
        def f2_tile(t):
            rows = slice(t * 128, (t + 1) * 128)
            h1, b_h1 = h1r.next()
            load(h1, h1_s[rows, :], b_h1, reads=[sb_h1])
            yield
            rstd, nmr, b_sm = ln_stats(h1, b_h1, None, smring)
            yield
            hn, b_hn = hnr.next()
            ACT(hn, h1, AF.Identity, [b_h1, b_sm], [b_hn], bias=nmr, scale=rstd)
            yield
            tt_, b_tt = tr_.next()
            TT('dve', tt_, hn, modbc[:, 4096:5120], ALU.mult, [b_hn, b_mod], [b_tt])
            yield
            u2, b_u2 = u2r.next()
            TT('pool', u2, tt_, modbc[:, 3072:4096], ALU.add, [b_tt, b_mod], [b_u2])
            yield
            u2b, b_u2b = u2br.next()
            ACT(u2b, u2, AF.Copy, [b_u2], [b_u2b])
            yield
            ps, b_ps = psn()
            psb = ps[:, :].bitcast(BF16)
            for kc in range(8):
                TR(psb[:, kc * 128:(kc + 1) * 128], u2b[:, kc * 128:(kc + 1) * 128], identb, [b_u2b, b_identb], [b_ps], part=(kc > 0))
            yield
            S.op('dve', lambda e, t=t, psb=psb: e.tensor_copy(u2T[:, :, t * 128:(t + 1) * 128],
                                                               psb.rearrange("p (a b) -> p a b", a=8)),
                 reads=[b_ps], writes=[b_u2T[t]])
            yield
            u32, b_u32 = u2t32.next()
            for half in range(2):
                ps, b_ps = psn()
                for kc in range(4):
                    TR(ps[:, kc * 128:(kc + 1) * 128], u2[:, (half * 4 + kc) * 128:(half * 4 + kc + 1) * 128], identf,
                       [b_u2, b_identf], [b_ps], part=(kc > 0))
                ACT(u32[:, half * 4:(half + 1) * 4, :], ps[:, :].rearrange("p (a b) -> p a b", a=4), AF.Copy, [b_ps], [b_u32],
                    part=(half > 0))
            yield
            ps, b_ps = psn()
            for kc in range(8):
                MM(ps[:, 0:36], u32[:, kc, :], wrt[:, kc, :], kc == 0, kc == 7, [b_u32, b_wrt], [b_ps])
            yield
            rt, b_rt = rtr.next()
            TT('dve', rt[:, 0:36], ps[:, 0:36], brt, ALU.add, [b_ps, b_brt], [b_rt])
            S.op('dve', lambda e, rt=rt: e.tensor_reduce(rt[:, 36:37], rt[:, 0:4], AX.X, ALU.max), reads=[b_rt], writes=[b_rt],
                 part=True)
            TS('dve', rt[:, 37:38], rt[:, 36:37], -1.0, None, ALU.mult, None, [b_rt], [b_rt], part=True)
            TS('dve', rt[:, 40:44], rt[:, 0:4], rt[:, 36:37], None, ALU.is_equal, None, [b_rt], [b_rt], part=True)
            yield
            ACT(rt[:, 81:85], rt[:, 0:4], AF.Exp, [b_rt], [b_rt], bias=rt[:, 37:38], scale=1.0, accum=rt[:, 38:39], part=True)
            S.op('dve', lambda e, rt=rt: e.reciprocal(rt[:, 39:40], rt[:, 38:39]), reads=[b_rt], writes=[b_rt], part=True)
            TS('dve', rt[:, 44:52], rt[:, 4:12], rt[:, 40:41], None, ALU.mult, None, [b_rt], [b_rt], part=True)
            for g in range(1, 4):
                STT(rt[:, 44:52], rt[:, 4 + 8 * g:12 + 8 * g], rt[:, 40 + g:41 + g], rt[:, 44:52], ALU.mult, ALU.add, [b_rt], [b_rt],
                    part=True)
            yield
            S.op('dve', lambda e, rt=rt: e.max(rt[:, 52:60], rt[:, 44:52]), reads=[b_rt], writes=[b_rt], part=True)
            TS('dve', rt[:, 60:61], rt[:, 52:53], -1.0, None, ALU.mult, None, [b_rt], [b_rt], part=True)
            yield
            ACT(rt[:, 61:62], rt[:, 53:54], AF.Exp, [b_rt], [b_rt], bias=rt[:, 60:61], scale=1.0, part=True)
            TS('dve', rt[:, 62:63], rt[:, 61:62], 1.0, None, ALU.add, None, [b_rt], [b_rt], part=True)
            S.op('dve', lambda e, rt=rt: e.reciprocal(rt[:, 62:63], rt[:, 62:63]), reads=[b_rt], writes=[b_rt], part=True)
            TT('dve', rt[:, 63:64], rt[:, 62:63], rt[:, 39:40], ALU.mult, [b_rt], [b_rt], part=True)
            TT('dve', rt[:, 64:65], rt[:, 63:64], rt[:, 61:62], ALU.mult, [b_rt], [b_rt], part=True)
            yield
            TS('dve', rt[:, 65:73], rt[:, 44:52], rt[:, 52:53], rt[:, 63:64], ALU.is_equal, ALU.mult, [b_rt], [b_rt], part=True)
            TS('dve', rt[:, 73:81], rt[:, 44:52], rt[:, 53:54], rt[:, 64:65], ALU.is_equal, ALU.mult, [b_rt], [b_rt], part=True)
            TT('dve', rt[:, 73:81], rt[:, 73:81], rt[:, 65:73], ALU.add, [b_rt], [b_rt], part=True)
            for g in range(4):
                TS('dve', gate32[:, t, 8 * g:8 * g + 8], rt[:, 73:81], rt[:, 40 + g:41 + g], None, ALU.mult, None, [b_rt],
                   [b_gate[t]], part=(g > 0))
        interleave((f2_tile(t) for t in range(NT_OWN)), 3)
        S.op('dve', lambda e: e.tensor_copy(gate2bc, modbc[:, 5120:6144]), reads=[b_mod], writes=[b_g2])
        S.barrier()
        arF.p, arB.p = pF_mod, pB_g

        if stop_after == 'F2':
            return finish()
        acc = arF.alloc([128, NT_OWN, D]); b_acc = [[S.buf('acc%d_%d' % (t, hf)) for hf in range(2)] for t in range(NT_OWN)]
        sar = Ring(arF, 'sa', [128, 512], 2)
        wslots = Ring(arB, 'wexp', [128, 4096], 5)
        hTr = Ring(arB, 'hT', [128, 4, 512], 2)
        pend = []

        def issue_e(e):
            res = []
            for nm_, src in (('w1', w1_d[e].rearrange("(kc k) n -> k kc n", k=128)),
                             ('w3', w3_d[e].rearrange("(kc k) n -> k kc n", k=128)),
                             ('w2', w2_d[e].rearrange("(kc k) n -> k kc n", k=128))):
                wt, b_wt = wslots.next()
                if nm_ == 'w2':
                    v = wt.rearrange("p (a b) -> p a b", a=4)
                else:
                    v = wt.rearrange("p (a b) -> p a b", a=8)
                res.append((v, b_wt, src))
            return res

        def do_load(item):
            v, b_wt, src = item
            load(v, src, b_wt, cast=True)

        cur = issue_e(0)
        for it in cur:
            do_load(it)

        def stage_c(e, tb, hT, b_hT, w2t, b_w2):
            for tt in range(4):
                t = tb * 4 + tt
                for hf in range(2):
                    psY, b_psY = psn()
                    for fc in range(4):
                        MM(psY[:, :], hT[:, fc, tt * 128:(tt + 1) * 128], w2t[:, fc, hf * 512:(hf + 1) * 512], fc == 0, fc == 3,
                           [b_hT, b_w2], [b_psY])
                    dst = acc[:, t, hf * 512:(hf + 1) * 512]
                    if e == 0:
                        TS('dve', dst, psY[:, :], gate32[:, t, e:e + 1], None, ALU.mult, None, [b_psY, b_gate[t]],
                           [b_acc[t][hf]])
                    else:
                        STT(dst, psY[:, :], gate32[:, t, e:e + 1], dst, ALU.mult, ALU.add, [b_psY, b_gate[t], b_acc[t][hf]],
                            [b_acc[t][hf]])

        pend_c = None
        for e in range(32):
            (w1t, b_w1, _), (w3t, b_w3, _), (w2t, b_w2, _) = cur
            nxt = issue_e(e + 1) if e + 1 < 32 else None
            for tb in range(4):
                hT, b_hT = hTr.next()
                toks = slice(tb * 512, (tb + 1) * 512)
                for fc in range(4):
                    psA, b_psA = psn()
                    for kc in range(8):
                        MM(psA[:, :], w1t[:, kc, fc * 128:(fc + 1) * 128], u2T[:, kc, toks], kc == 0, kc == 7,
                           [b_w1] + b_u2T[tb * 4:tb * 4 + 4], [b_psA])
                    psBk, b_psBk = psn()
                    for kc in range(8):
                        MM(psBk[:, :], w3t[:, kc, fc * 128:(fc + 1) * 128], u2T[:, kc, toks], kc == 0, kc == 7,
                           [b_w3] + b_u2T[tb * 4:tb * 4 + 4], [b_psBk])
                    sa, b_sa = sar.next()
                    ACT(sa, psA[:, :], AF.Silu, [b_psA], [b_sa])
                    TT('dve', hT[:, fc, :], sa, psBk[:, :], ALU.mult, [b_sa, b_psBk], [b_hT], part=(fc > 0))
                if pend_c is not None:
                    stage_c(*pend_c)
                pend_c = (e, tb, hT, b_hT, w2t, b_w2)
                if tb == 0 and nxt:
                    do_load(nxt[0])
                    do_load(nxt[1])
            if nxt:
                do_load(nxt[2])
            cur = nxt
        stage_c(*pend_c)

        if stop_after == 'G':
            return finish()
        ln2g = arF.alloc([128, D]); b_ln2g = S.buf('ln2g')
        ln2b = arF.alloc([128, D]); b_ln2b = S.buf('ln2b')
        load(ln2g, ln2g_d[:, :], b_ln2g)
        load(ln2b, ln2b_d[:, :], b_ln2b)
        h1r = Ring(arF, 'h1H', [128, D], 2)
        outr = Ring(arF, 'oH', [128, D], 2)
        smring = Ring(arF, 'smH', [128, 32], 4)
        b_out = S.buf('out')
        for t in range(NT_OWN):
            rows = slice(t * 128, (t + 1) * 128)
            h1, b_h1 = h1r.next()
            load(h1, h1_s[rows, :], b_h1, reads=[sb_h1])
            ot, b_ot = outr.next()
            a_t = acc[:, t, :]
            ba = b_acc[t]
            TT('dve', ot, a_t, gate2bc, ALU.mult, ba + [b_g2], [b_ot])
            for hf in range(2):
                cs = slice(hf * 512, (hf + 1) * 512)
                STT(a_t[:, cs], h1[:, cs], ALPHA, ot[:, cs], ALU.mult, ALU.add, [b_h1, b_ot], [ba[hf]])
            sm, b_sm = smring.next()
            for j in range(2):
                S.op('dve', lambda e, j=j, sm=sm, a_t=a_t: e.bn_stats(sm[:, 6 * j:6 * j + 6], a_t[:, 512 * j:512 * (j + 1)]),
                     reads=[ba[j]], writes=[b_sm], part=(j > 0))
            S.op('dve', lambda e, sm=sm: e.bn_aggr(sm[:, 12:14], sm[:, 0:12]), reads=[b_sm], writes=[b_sm], part=True)
            ACT(sm[:, 14:15], sm[:, 13:14], AF.Sqrt, [b_sm, b_epsc], [b_sm], bias=epsc[:, 0:1], scale=1.0, part=True)
            S.op('dve', lambda e, sm=sm: e.reciprocal(sm[:, 15:16], sm[:, 14:15]), reads=[b_sm], writes=[b_sm], part=True)
            STT(sm[:, 16:17], sm[:, 12:13], -1.0, sm[:, 15:16], ALU.mult, ALU.mult, [b_sm], [b_sm], part=True)
            ACT(ot, a_t, AF.Identity, ba + [b_sm], [b_ot], bias=sm[:, 16:17], scale=sm[:, 15:16])
            TT('dve', ot, ot, ln2g, ALU.mult, [b_ot, b_ln2g], [b_ot])
            TT('pool', ot, ot, ln2b, ALU.add, [b_ot, b_ln2b], [b_ot])
            S.op('pool', lambda e, ot=ot, rows=rows: e.dma_start(out=out_d[rows, :], in_=ot), reads=[b_ot], writes=[b_out],
                 part=True, dma=True, sembuf=b_ot)
        S.barrier()
        blk = st.enter_context(nc.Block())
        S.emit(blk)
    return nc


_NC_CACHE = {}


def _host_tables():
    import ml_dtypes
    slopes = np.exp2(-8.0 * np.arange(1, 13, dtype=np.float32) / 12.0).astype(np.float32)
    i = np.arange(128)[:, None]
    j = np.arange(128)[None, :]
    ab = np.zeros((128, 12, 4, 128), np.float32)
    for h in range(12):
        d = (1, 4, 16)[h // 4]
        dA = j - 64 - i
        dB = j + 64 - i
        dF = j - i
        tA = np.where(np.abs(dA) <= 64, -slopes[h] * d * np.abs(dA), NEG)
        tB = np.where(np.abs(dB) <= 64, -slopes[h] * d * np.abs(dB), NEG)
        tF = np.where((np.abs(dF) <= 64) & (j < 64), -slopes[h] * d * np.abs(dF), NEG)
        ab[:, h, 0] = tA
        ab[:, h, 1] = tB
        ab[:, h, 2] = tF
        ab[:, h, 3] = tB
    ab = ab.reshape(128, 12 * 4 * 128).astype(np.float32)
    m = np.arange(128, dtype=np.float32)[:, None]
    n = np.arange(128, dtype=np.float32)[None, :]
    rt = np.zeros((128, 4 * 128 + 8), np.float32)
    rt[:, 0:128] = np.maximum(n - m, 0)
    rt[:, 128:256] = np.maximum(m - n, 0)
    rt[:, 256:384] = n + 1
    rt[:, 384:512] = 128 - n
    rt[:, 512] = 127 - m[:, 0]
    rt[:, 513] = m[:, 0]
    rt[:, 514] = 128
    identf = np.eye(128, dtype=np.float32)
    identb = np.eye(128, dtype=np.float32).astype(ml_dtypes.bfloat16)
    return ab, rt, identf, identb


def _prep(x, c, w_ada, b_ada, w_in, w_attn_out, ret_decay_fwd, ret_decay_bwd, ret_gn_gain,
          w_ret_out, w_out, ln1_gain, ln1_bias, w_coarse, b_coarse, w_fine, b_fine,
          w1, w3, w2, ln2_gain, ln2_bias, ne=32, cores=range(8)):
    f = lambda a: np.ascontiguousarray(np.asarray(a, dtype=np.float32))
    x = f(x); c = f(c)
    ab, rt, identf, identb = _host_tables()
    rep = lambda v: np.ascontiguousarray(np.broadcast_to(f(v).reshape(1, -1), (128, f(v).size)))
    wrt = np.ascontiguousarray(np.concatenate([f(w_coarse)[0], f(w_fine)[0].transpose(1, 0, 2).reshape(D, 32)], axis=1))
    brt = rep(np.concatenate([f(b_coarse)[0].reshape(-1), f(b_fine)[0].reshape(-1)]))
    shared = {
        "w_ada": f(w_ada)[0], "bada": rep(b_ada[0]), "w_in": f(w_in)[0], "w_attn_out": f(w_attn_out)[0],
        "w_ret_out": f(w_ret_out)[0], "w_out": f(w_out)[0], "gng": rep(ret_gn_gain[0]),
        "ln1g": rep(ln1_gain[0]), "ln1b": rep(ln1_bias[0]), "ln2g": rep(ln2_gain[0]), "ln2b": rep(ln2_bias[0]),
        "wrt": wrt, "brt": brt, "w1": f(w1)[0][:ne], "w3": f(w3)[0][:ne], "w2": f(w2)[0][:ne],
        "abias": ab, "rtab": rt, "identf": identf, "identb": identb,
    }
    lf = f(ret_decay_fwd)[0]
    lb = f(ret_decay_bwd)[0]
    in_maps = []
    for core in cores:
        b, half = core // 2, core % 2
        xc = x[b] if half == 0 else x[b, ::-1]
        cb = np.ascontiguousarray(np.broadcast_to(c[b].reshape(8, 128).T[:, :, None], (128, 8, 128)))
        rl = np.concatenate([lf, lb]) if half == 0 else np.concatenate([lb, lf])
        m = dict(shared)
        m["x"] = np.ascontiguousarray(xc)
        m["cbc"] = cb
        m["retlog"] = rep(rl)
        in_maps.append(m)
    return in_maps


def kernel(**inputs):
    if 'nc' not in _NC_CACHE:
        _NC_CACHE['nc'] = build_nc()
    nc = _NC_CACHE['nc']
    in_maps = _prep(**inputs)
    res = run_bass_kernel_spmd(nc, in_maps, core_ids=list(range(8)))
    out = np.zeros((4, S_ALL, D), np.float32)
    for core in range(8):
        b, half = core // 2, core % 2
        o = np.asarray(res.results[core]["out"], dtype=np.float32)
        if half == 0:
            out[b, :S_OWN] = o
        else:
            out[b, S_OWN:] = o[::-1]
    return out
```

```python
import numpy as np
from contextlib import ExitStack
import concourse.bass as bass
import concourse.mybir as mybir
from concourse.bass_utils import run_bass_kernel_spmd

F32 = mybir.dt.float32
BF16 = mybir.dt.bfloat16
ALU = mybir.AluOpType
AF = mybir.ActivationFunctionType
AX = mybir.AxisListType

D = 1024
S_ALL = 4096
S_OWN = 2048
NT_OWN = 16
NT_ALL = 32
ALPHA = 2.0 ** 0.25
EPS = 1e-5
NEG = -30000.0
DEBUG_OUT = []


class Buf:
    __slots__ = ('name', 'w', 'r', 'sem', 'semval', 'kind')

    def __init__(self, name):
        self.name = name
        self.w = {}
        self.r = {}
        self.sem = None
        self.semval = 0


class Sched:
    ENG = ['pe', 'dve', 'act', 'pool', 'sp']

    def __init__(self, nc, stack):
        self.nc = nc
        self.stack = stack
        self.e = {}
        for name in self.ENG:
            sem = stack.enter_context(nc.semaphore(name + '_sem'))
            self.e[name] = dict(sem=sem, n=0, waited={}, prog=[], pending=[])
        self.bufs = []
        self.free = {'sw': [], 'hw': []}
        self.nsem = 0

    def buf(self, name):
        b = Buf(name)
        self.bufs.append(b)
        return b

    def _bufsem(self, b, eng='sp'):
        kind = 'sw' if eng == 'pool' else 'hw'
        if b.sem is not None:
            assert b.kind == kind, b.name
        if b.sem is None:
            b.kind = kind
            if self.free[kind]:
                b.sem, b.semval = self.free[kind].pop()
            else:
                b.sem = self.stack.enter_context(self.nc.semaphore('d%d' % self.nsem))
                b.semval = 0
                self.nsem += 1
        return b.sem

    def op(self, eng, fn, reads=(), writes=(), part=False, dma=False, sembuf=None):
        E = self.e[eng]
        deps = []
        for b in reads:
            for d in b.w.values():
                deps.append((d, 0))
        for b in writes:
            if not part:
                for d in b.w.values():
                    deps.append((d, 1))
            for d in b.r.values():
                deps.append((d, 2))
        waits = E['pending']
        E['pending'] = []
        for (sem, val, src, is_dma), kind in deps:
            if not is_dma and src == eng:
                if eng == 'pe' or kind == 2:
                    continue
            k = id(sem)
            if E['waited'].get(k, 0) >= val:
                continue
            E['waited'][k] = val
            waits.append((sem, val))
        if dma:
            dst = sembuf if sembuf is not None else writes[0]
            sem = self._bufsem(dst, eng)
            dst.semval += 16
            ev = (sem, dst.semval, eng, True)
            inc = 16
        else:
            E['n'] += 1
            sem = E['sem']
            ev = (sem, E['n'], eng, False)
            inc = 1
        k = id(sem)
        for b in writes:
            if not part:
                b.w = {}
                b.r = {}
            b.w[k] = ev
        for b in reads:
            b.r[k] = ev
        E['prog'].append((waits, fn, sem, inc))

    def barrier(self):
        evs = {}
        for b in self.bufs:
            for dd in (b.w, b.r):
                for k, d in dd.items():
                    if k not in evs or evs[k][1] < d[1]:
                        evs[k] = d
        for name in self.ENG:
            E = self.e[name]
            for k, (sem, val, src, is_dma) in evs.items():
                if E['waited'].get(k, 0) >= val:
                    continue
                if not is_dma and src == name:
                    continue
                E['waited'][k] = val
                E['pending'].append((sem, val))
        for b in self.bufs:
            if b.sem is not None:
                self.free[b.kind].append((b.sem, b.semval))
                b.sem = None

    def emit(self, block):
        sched = self

        def run(engname, eng):
            E = sched.e[engname]
            for waits, fn, sem, inc in E['prog']:
                for s, v in waits:
                    eng.wait_ge(s, v)
                ins = fn(eng)
                ins.then_inc(sem, inc)
            for s, v in E['pending']:
                eng.wait_ge(s, v)

        @block.tensor
        def _(eng):
            run('pe', eng)

        @block.vector
        def _(eng):
            run('dve', eng)

        @block.scalar
        def _(eng):
            run('act', eng)

        @block.gpsimd
        def _(eng):
            run('pool', eng)

        @block.sync
        def _(eng):
            run('sp', eng)


class Arena:
    def __init__(self, t, n):
        self.t = t
        self.n = n
        self.p = 0

    def alloc(self, shape):
        sz = 1
        for s in shape[1:]:
            sz *= s
        sz = (sz + 15) // 16 * 16
        a = self.p
        self.p += sz
        assert self.p <= self.n, ('arena overflow', self.p, self.n)
        ap = self.t[0:shape[0], a:a + int(np.prod(shape[1:]))]
        if len(shape) == 3:
            ap = ap.rearrange("p (a b) -> p a b", a=shape[1])
        return ap


def build_nc(stop_after=None, ne=32):
    nc = bass.Bass("TRN2", target_bir_lowering=False)

    def din(name, shape, dt=F32):
        return nc.dram_tensor(name, shape, dt, kind="ExternalInput").ap()

    def dscr(name, shape, dt):
        kind = "ExternalOutput" if name in DEBUG_OUT else "Internal"
        return nc.dram_tensor(name, shape, dt, kind=kind).ap()

    x_d = din("x", [S_ALL, D])
    cbc_d = din("cbc", [128, 8, 128])
    wada_d = din("w_ada", [D, 6 * D])
    bada_d = din("bada", [128, 6 * D])
    win_d = din("w_in", [D, 12800])
    wa_d = din("w_attn_out", [512, D])
    wr_d = din("w_ret_out", [2048, D])
    wo_d = din("w_out", [D, D])
    retlog_d = din("retlog", [128, 8])
    gng_d = din("gng", [128, 2048])
    ln1g_d = din("ln1g", [128, D])
    ln1b_d = din("ln1b", [128, D])
    ln2g_d = din("ln2g", [128, D])
    ln2b_d = din("ln2b", [128, D])
    wrt_d = din("wrt", [D, 36])
    brt_d = din("brt", [128, 36])
    w1_d = din("w1", [ne, D, 512])
    w3_d = din("w3", [ne, D, 512])
    w2_d = din("w2", [ne, 512, D])
    abias_d = din("abias", [128, 12 * 4 * 128])
    rtab_d = din("rtab", [128, 4 * 128 + 8])
    identf_d = din("identf", [128, 128])
    identb_d = din("identb", [128, 128], BF16)
    out_d = nc.dram_tensor("out", [S_OWN, D], F32, kind="ExternalOutput").ap()

    qTa_s = dscr("qTa_s", [12, 128, S_OWN], BF16)
    kTa_s = dscr("kTa_s", [12, 128, 3072], BF16)
    va_s = dscr("va_s", [3072, 1536], BF16)
    qTr_s = dscr("qTr_s", [8, 128, S_OWN], BF16)
    kTr_s = dscr("kTr_s", [8, 128, S_OWN], BF16)
    krF_s = dscr("krF_s", [S_OWN, 1024], BF16)
    krB_s = dscr("krB_s", [S_ALL, 1024], BF16)
    vr_s = dscr("vr_s", [S_ALL, 2048], BF16)
    srg_s = dscr("srg_s", [S_OWN, 2048], BF16)
    sga_s = dscr("sga_s", [S_OWN, 1024], BF16)
    sgr_s = dscr("sgr_s", [S_OWN, 1024], BF16)
    ao_s = dscr("ao_s", [S_OWN, 12, 130], F32)
    retg_s = dscr("retg_s", [S_OWN, 2048], BF16)
    h1_s = dscr("h1_s", [S_OWN, D], F32)

    with ExitStack() as st:
        S = Sched(nc, st)
        def finish():
            S.barrier()
            blk = st.enter_context(nc.Block())
            S.emit(blk)
            return nc
        NF, NB = 26624, 45056
        arF_t = st.enter_context(nc.sbuf_tensor("arF", [128, NF], F32))
        arB_t = st.enter_context(nc.sbuf_tensor("arB", [128, NB], BF16))
        arF = Arena(arF_t, NF)
        arB = Arena(arB_t, NB)
        PS = [st.enter_context(nc.psum_tensor("ps%d" % i, [128, 512], F32)) for i in range(8)]
        PB = [S.buf("ps%d" % i) for i in range(8)]
        psi = [0]

        def psn():
            i = psi[0] % 8
            psi[0] += 1
            return PS[i], PB[i]

        class Ring:
            def __init__(self, ar, name, shape, n):
                self.t = [ar.alloc(shape) for _ in range(n)]
                self.b = [S.buf(name + str(i)) for i in range(n)]
                self.i = 0
                self.n = n

            def next(self):
                k = self.i % self.n
                self.i += 1
                return self.t[k], self.b[k]

        def load(dst, src, b, part=False, cast=False, reads=()):
            q = 'pool' if cast else 'sp'
            S.op(q, lambda e: e.dma_start(out=dst, in_=src), reads=list(reads), writes=[b], part=part, dma=True)

        def store(dst, src, db, sb_, q='sp'):
            S.op(q, lambda e: e.dma_start(out=dst, in_=src), reads=[sb_], writes=[db], part=True, dma=True, sembuf=sb_)

        def TT(eng, out, a, b, op, reads, writes, part=False):
            S.op(eng, lambda e: e.tensor_tensor(out, a, b, op), reads=reads, writes=writes, part=part)

        def TS(eng, out, a, s1, s2, op0, op1, reads, writes, part=False):
            if op1 is None:
                S.op(eng, lambda e: e.tensor_scalar(out, a, s1, None, op0), reads=reads, writes=writes, part=part)
            else:
                S.op(eng, lambda e: e.tensor_scalar(out, a, s1, s2, op0, op1), reads=reads, writes=writes, part=part)

        def STT(out, a, s, b, op0, op1, reads, writes, part=False):
            S.op('dve', lambda e: e.scalar_tensor_tensor(out, a, s, b, op0, op1), reads=reads, writes=writes, part=part)

        def ACT(out, in_, func, reads, writes, bias=None, scale=None, accum=None, part=False):
            kw = {}
            if bias is not None:
                kw['bias'] = bias
            if scale is not None:
                kw['scale'] = scale
            if accum is not None:
                kw['accum_out'] = accum
            S.op('act', lambda e: e.activation(out, in_, func, **kw), reads=reads, writes=writes, part=part)

        def MM(out, lhsT, rhs, start, stop, reads, writes):
            S.op('pe', lambda e: e.matmul(out, lhsT, rhs, start=start, stop=stop), reads=reads, writes=writes,
                 part=not start)

        def TR(out, in_, ident, reads, writes, part):
            S.op('pe', lambda e: e.transpose(out, in_, ident), reads=reads, writes=writes, part=part)

        pF0 = arF.p
        identf = arF.alloc([128, 128]); b_identf = S.buf('identf')
        load(identf, identf_d[:, :], b_identf)
        epsc = arF.alloc([128, 16]); b_epsc = S.buf('epsc')
        S.op('dve', lambda e: e.memset(epsc[:, 0:1], EPS), writes=[b_epsc])
        kdt = arF.alloc([128, 16]); b_kdt = S.buf('kdt')
        lgt = arF.alloc([128, 16]); b_lgt = S.buf('lgt')
        rtab = arF.alloc([128, 4 * 128 + 8]); b_rtab = S.buf('rtab')
        load(rtab, rtab_d[:, :], b_rtab)
        gate2bc = arF.alloc([128, D]); b_g2 = S.buf('gate2bc')
        gate32 = arF.alloc([128, NT_OWN, 32]); b_gate = [S.buf('gate%d' % t) for t in range(NT_OWN)]
        pF_mod = arF.p
        modbc = arF.alloc([128, 6 * D]); b_mod = S.buf('modbc')
        pB0 = arB.p
        identb = arB.alloc([128, 128]); b_identb = S.buf('identb')
        load(identb, identb_d[:, :], b_identb)
        pF_base, pB_base = arF.p, arB.p

        def ln_stats(src, b_src, rstd_ring, small_ring):
            n = src.shape[1]
            sm, b_sm = small_ring.next()
            nst = n // 512
            for j in range(nst):
                S.op('dve', lambda e, j=j: e.bn_stats(sm[:, 6 * j:6 * j + 6], src[:, 512 * j:512 * (j + 1)]),
                     reads=[b_src], writes=[b_sm], part=(j > 0))
            S.op('dve', lambda e: e.bn_aggr(sm[:, 12:14], sm[:, 0:6 * nst]), reads=[b_sm], writes=[b_sm], part=True)
            ACT(sm[:, 14:15], sm[:, 13:14], AF.Sqrt, [b_sm, b_epsc], [b_sm], bias=epsc[:, 0:1], scale=1.0, part=True)
            S.op('dve', lambda e: e.reciprocal(sm[:, 15:16], sm[:, 14:15]), reads=[b_sm], writes=[b_sm], part=True)
            STT(sm[:, 16:17], sm[:, 12:13], -1.0, sm[:, 15:16], ALU.mult, ALU.mult, [b_sm], [b_sm], part=True)
            return sm[:, 15:16], sm[:, 16:17], b_sm

        def interleave(gens, width):
            active = []
            it = iter(gens)
            while True:
                if len(active) < width:
                    g_ = next(it, None)
                    if g_ is not None:
                        active.append(g_)
                if not active:
                    break
                for g_ in list(active):
                    try:
                        next(g_)
                    except StopIteration:
                        active.remove(g_)

        cbc = arF.alloc([128, 8, 128]); b_cbc = S.buf('cbc')
        load(cbc, cbc_d[:, :, :], b_cbc)
        wring = Ring(arF, 'wada', [128, 8, 512], 2)
        bring = Ring(arF, 'bada', [128, 512], 2)
        for blk in range(12):
            wt, b_wt = wring.next()
            bt, b_bt = bring.next()
            load(wt, wada_d[:, blk * 512:(blk + 1) * 512].rearrange("(kc k) n -> k kc n", k=128), b_wt)
            load(bt, bada_d[:, blk * 512:(blk + 1) * 512], b_bt)
            ps, b_ps = psn()
            for kc in range(8):
                MM(ps[:, :], cbc[:, kc, :], wt[:, kc, :], kc == 0, kc == 7, [b_cbc, b_wt], [b_ps])
            add1 = 1.0 if blk in (2, 3, 8, 9) else 0.0
            STT(modbc[:, blk * 512:(blk + 1) * 512], ps[:, :], add1, bt, ALU.add, ALU.add, [b_ps, b_bt], [b_mod],
                part=(blk > 0))
        retlog = arF.alloc([128, 8]); b_retlog = S.buf('retlog')
        load(retlog, retlog_d[:, :], b_retlog)
        ACT(lgt[:, 8:16], retlog, AF.Exp, [b_retlog], [b_lgt], scale=-1.0)
        ACT(lgt[:, 8:16], lgt[:, 8:16], AF.Ln, [b_lgt], [b_lgt], bias=1.0, scale=1.0, part=True)
        TS('dve', lgt[:, 0:8], lgt[:, 8:16], -1.0, None, ALU.mult, None, [b_lgt], [b_lgt], part=True)
        for h in range(4):
            ACT(kdt[:, h:h + 1], rtab[:, 512:513], AF.Exp, [b_rtab, b_lgt], [b_kdt], scale=lgt[:, h:h + 1], part=(h > 0))
            ACT(kdt[:, 4 + h:5 + h], rtab[:, 513:514], AF.Exp, [b_rtab, b_lgt], [b_kdt], scale=lgt[:, 4 + h:5 + h], part=True)
            ACT(kdt[:, 8 + h:9 + h], rtab[:, 514:515], AF.Exp, [b_rtab, b_lgt], [b_kdt], scale=lgt[:, h:h + 1], part=True)
            ACT(kdt[:, 12 + h:13 + h], rtab[:, 514:515], AF.Exp, [b_rtab, b_lgt], [b_kdt], scale=lgt[:, 4 + h:5 + h], part=True)
        TS('dve', kdt[:, 0:8], kdt[:, 0:8], 0.0625, None, ALU.mult, None, [b_kdt], [b_kdt], part=True)
        S.barrier()
        arF.p, arB.p = pF_base, pB_base

        if stop_after == 'A':
            return finish()
        uT = arB.alloc([128, 8, S_ALL])
        b_uT = [S.buf('uT%d' % t) for t in range(NT_ALL)]
        pB_c = arB.p
        xring = Ring(arF, 'x', [128, D], 5)
        xnring = Ring(arF, 'xn', [128, D], 4)
        t1ring = Ring(arF, 't1', [128, D], 4)
        smring = Ring(arF, 'sm', [128, 32], 8)
        ubring = Ring(arB, 'ub', [128, D], 4)

        def phB_tile(t):
            xs, b_xs = xring.next()
            load(xs, x_d[t * 128:(t + 1) * 128, :], b_xs)
            yield
            rstd, nmr, b_sm = ln_stats(xs, b_xs, None, smring)
            yield
            xn, b_xn = xnring.next()
            ACT(xn, xs, AF.Identity, [b_xs, b_sm], [b_xn], bias=nmr, scale=rstd)
            yield
            t1, b_t1 = t1ring.next()
            TT('dve', t1, xn, modbc[:, 1024:2048], ALU.mult, [b_xn, b_mod], [b_t1])
            yield
            ub, b_ub = ubring.next()
            TT('pool', ub, t1, modbc[:, 0:1024], ALU.add, [b_t1, b_mod], [b_ub])
            yield
            ps, b_ps = psn()
            psb = ps[:, :].bitcast(BF16)
            for kc in range(8):
                TR(psb[:, kc * 128:(kc + 1) * 128], ub[:, kc * 128:(kc + 1) * 128], identb, [b_ub, b_identb], [b_ps],
                   part=(kc > 0))
            yield
            ACT(uT[:, :, t * 128:(t + 1) * 128], psb.rearrange("p (a b) -> p a b", a=8), AF.Copy, [b_ps], [b_uT[t]])
        interleave((phB_tile(t) for t in range(NT_ALL)), 4)
        S.barrier()
        arF.p, arB.p = pF_base, pB_c

        if stop_after == 'B':
            return finish()
        wring = Ring(arB, 'win', [128, 8, 512], 2)
        stg = Ring(arB, 'stg', [128, 512], 6)
        sb_q = S.buf('qTa_s'); sb_k = S.buf('kTa_s'); sb_v = S.buf('va_s')
        sb_qr = S.buf('qTr_s'); sb_kr = S.buf('kTr_s'); sb_krF = S.buf('krF_s'); sb_krB = S.buf('krB_s')
        sb_vr = S.buf('vr_s'); sb_srg = S.buf('srg_s'); sb_sga = S.buf('sga_s'); sb_sgr = S.buf('sgr_s')
        evc = [0]

        def evac_copy(dst, src, reads, writes, scale=None):
            evc[0] += 1
            if evc[0] % 2 == 0:
                ACT(dst, src, AF.Copy, reads, writes, scale=scale)
            elif scale is not None:
                TS('dve', dst, src, scale, None, ALU.mult, None, reads, writes)
            else:
                S.op('dve', lambda e: e.tensor_copy(dst, src), reads=reads, writes=writes)

        blocks = []
        for i in range(3):
            blocks.append((i * 512, 'aq'))
        for i in range(3):
            blocks.append((1536 + i * 512, 'ak'))
        for i in range(3):
            blocks.append((3072 + i * 512, 'av'))
        for i in range(2):
            blocks.append((4608 + i * 512, 'rq'))
        for i in range(2):
            blocks.append((5632 + i * 512, 'rk'))
        for i in range(4):
            blocks.append((6656 + i * 512, 'rv'))
        for i in range(4):
            blocks.append((8704 + i * 512, 'rg'))
        for i in range(2):
            blocks.append((10752 + i * 512, 'ga'))
        for i in range(2):
            blocks.append((11776 + i * 512, 'gr'))
        import os as _os
        if _os.environ.get('C_KINDS'):
            blocks = [b for b in blocks if b[1] in _os.environ['C_KINDS'].split(',')]
        wt_list = []

        def issue_w(j):
            c0, _ = blocks[j]
            wt, b_wt = wring.next()
            load(wt, win_d[:, c0:c0 + 512].rearrange("(kc k) n -> k kc n", k=128), b_wt, cast=True)
            wt_list.append((wt, b_wt))

        def fm(wt, b_wt, j, ntb, dst_fn, b_dst, scale=None):
            for tb in range(ntb):
                ps, b_ps = psn()
                for kc in range(8):
                    MM(ps[:, :], wt[:, kc, j * 128:(j + 1) * 128], uT[:, kc, tb * 512:(tb + 1) * 512], kc == 0, kc == 7,
                       [b_wt] + b_uT[tb * 4:tb * 4 + 4], [b_ps])
                sg, b_sg = stg.next()
                evac_copy(sg, ps[:, :], [b_ps], [b_sg], scale=scale)
                store(dst_fn(tb), sg, b_dst, b_sg)

        def tm(wt, b_wt, tiles, evac_fn):
            for t in tiles:
                ps, b_ps = psn()
                for kc in range(8):
                    MM(ps[:, :], uT[:, kc, t * 128:(t + 1) * 128], wt[:, kc, :], kc == 0, kc == 7, [b_wt, b_uT[t]], [b_ps])
                evac_fn(t, ps, b_ps)

        issue_w(0)
        for bi, (c0, kind) in enumerate(blocks):
            if bi + 1 < len(blocks):
                issue_w(bi + 1)
            wt, b_wt = wt_list[bi]
            if kind == 'aq':
                for j in range(4):
                    ch = (c0 // 128) + j
                    fm(wt, b_wt, j, 4, lambda tb, ch=ch: qTa_s[ch, :, tb * 512:(tb + 1) * 512], sb_q)
            elif kind == 'ak':
                for j in range(4):
                    ch = ((c0 - 1536) // 128) + j
                    fm(wt, b_wt, j, (5, 5, 6)[(c0 - 1536) // 512], lambda tb, ch=ch: kTa_s[ch, :, tb * 512:(tb + 1) * 512], sb_k)
            elif kind == 'rq':
                for j in range(4):
                    ch = ((c0 - 4608) // 128) + j
                    fm(wt, b_wt, j, 4, lambda tb, ch=ch: qTr_s[ch, :, tb * 512:(tb + 1) * 512], sb_qr)
            elif kind == 'av':
                cc = c0 - 3072

                def ev(t, ps, b_ps, cc=cc):
                    sg, b_sg = stg.next()
                    evac_copy(sg, ps[:, :], [b_ps], [b_sg])
                    store(va_s[t * 128:(t + 1) * 128, cc:cc + 512], sg, sb_v, b_sg)
                tm(wt, b_wt, range((17, 18, 24)[cc // 512]), ev)
            elif kind == 'rk':
                cc = c0 - 5632
                rkm = _os.environ.get('RK_MODE', 'fm,tmB,tmF')
                for j in range(4 if 'fm' in rkm else 0):
                    ch = (cc // 128) + j
                    fm(wt, b_wt, j, 4, lambda tb, ch=ch: kTr_s[ch, :, tb * 512:(tb + 1) * 512], sb_kr, scale=0.0625)

                def ev(t, ps, b_ps, cc=cc):
                    h0 = cc // 256
                    sg, b_sg = stg.next()
                    for hh in range(2 if 'tmB' in rkm else 0):
                        TS('dve', sg[:, hh * 256:(hh + 1) * 256], ps[:, hh * 256:(hh + 1) * 256],
                           kdt[:, 4 + h0 + hh:5 + h0 + hh], None, ALU.mult, None, [b_ps, b_kdt], [b_sg], part=(hh > 0))
                    store(krB_s[t * 128:(t + 1) * 128, cc:cc + 512], sg, sb_krB, b_sg)
                    if t < NT_OWN and 'tmF' in rkm:
                        sg2, b_sg2 = stg.next()
                        for hh in range(2):
                            TS('dve', sg2[:, hh * 256:(hh + 1) * 256], ps[:, hh * 256:(hh + 1) * 256],
                               kdt[:, h0 + hh:h0 + hh + 1], None, ALU.mult, None, [b_ps, b_kdt], [b_sg2], part=(hh > 0))
                        store(krF_s[t * 128:(t + 1) * 128, cc:cc + 512], sg2, sb_krF, b_sg2)
                tm(wt, b_wt, range(NT_ALL), ev)
            elif kind == 'rv':
                cc = c0 - 6656

                def ev(t, ps, b_ps, cc=cc):
                    sg, b_sg = stg.next()
                    evac_copy(sg, ps[:, :], [b_ps], [b_sg])
                    store(vr_s[t * 128:(t + 1) * 128, cc:cc + 512], sg, sb_vr, b_sg)
                tm(wt, b_wt, range(NT_ALL), ev)
            else:
                base, dst, b_dst, func = {'rg': (8704, srg_s, sb_srg, AF.Silu), 'ga': (10752, sga_s, sb_sga, AF.Sigmoid),
                                          'gr': (11776, sgr_s, sb_sgr, AF.Sigmoid)}[kind]
                cc = c0 - base

                def ev(t, ps, b_ps, cc=cc, dst=dst, b_dst=b_dst, func=func):
                    sg, b_sg = stg.next()
                    ACT(sg, ps[:, :], func, [b_ps], [b_sg])
                    store(dst[t * 128:(t + 1) * 128, cc:cc + 512], sg, b_dst, b_sg)
                tm(wt, b_wt, range(NT_OWN), ev)
        S.barrier()
        arF.p, arB.p = pF_base, pB0 + 128
        pB_base2 = arB.p

        if stop_after == 'C':
            return finish()
        abias = arF.alloc([128, 12 * 4 * 128]); b_abias = S.buf('abias')
        load(abias, abias_d[:, :], b_abias)
        qring = Ring(arB, 'qh', [128, S_OWN], 2)
        kring = Ring(arB, 'kh', [128, 3072], 2)
        vring = Ring(arB, 'vt', [128, 128], 20)
        pring = Ring(arB, 'P', [128, 256], 8)
        ptring = Ring(arB, 'PT', [128, 256], 8)
        sbring = Ring(arF, 'Sb', [128, 256], 8)
        osring = Ring(arF, 'os', [128, 130], 10)
        sb_ao = S.buf('ao_s')

        def attn_item(h, d, r, i, qv, kv, aov, vav, b_qh, b_kh):
            n0 = 128 * i
            vts = []
            for j in (i, i + 1):
                vt, b_vt = vring.next()
                ks = 0 if j == 0 else 64 + 128 * (j - 1)
                load(vt, vav[r, ks:ks + 128, h * 128:(h + 1) * 128], b_vt, reads=[sb_v])
                vts.append((vt, b_vt))
            qa = qv[:, r, n0:n0 + 128]
            ps, b_ps = psn()
            for jj, j in enumerate((i, i + 1)):
                ks = 0 if j == 0 else 64 + 128 * (j - 1)
                MM(ps[:, jj * 128:(jj + 1) * 128], qa, kv[:, r, ks:ks + 128], True, True, [b_qh, b_kh], [b_ps])
            yield
            sbt, b_sbt = sbring.next()
            bo = (h * 4 + (2 if i == 0 else 0)) * 128
            STT(sbt, ps[:, 0:256], 128.0 ** -0.5, abias[:, bo:bo + 256], ALU.mult, ALU.add, [b_ps, b_abias], [b_sbt])
            ost, b_ost = osring.next()
            S.op('dve', lambda e: e.tensor_reduce(ost[:, 128:129], sbt, AX.X, ALU.max, negate=True),
                 reads=[b_sbt], writes=[b_ost])
            yield
            pt_, b_p = pring.next()
            ACT(pt_, sbt, AF.Exp, [b_sbt, b_ost], [b_p, b_ost], bias=ost[:, 128:129], scale=1.0,
                accum=ost[:, 129:130], part=True)
            yield
            ps2, b_ps2 = psn()
            ps2b = ps2[:, :].bitcast(BF16)
            for jj in range(2):
                TR(ps2b[:, jj * 128:(jj + 1) * 128], pt_[:, jj * 128:(jj + 1) * 128], identb, [b_p, b_identb],
                   [b_ps2], part=(jj > 0))
            yield
            ptt, b_ptt = ptring.next()
            S.op('dve', lambda e: e.tensor_copy(ptt, ps2b[:, 0:256]), reads=[b_ps2], writes=[b_ptt])
            yield
            ps3, b_ps3 = psn()
            for jj in range(2):
                vt, b_vt = vts[jj]
                MM(ps3[:, 0:128], ptt[:, jj * 128:(jj + 1) * 128], vt, jj == 0, jj == 1, [b_ptt, b_vt], [b_ps3])
            yield
            ACT(ost[:, 0:128], ps3[:, 0:128], AF.Copy, [b_ps3], [b_ost], part=True)
            store(aov[r, n0:n0 + 128, h, :], ost, sb_ao, b_ost, q='act')

        def attn_items():
            for h in range(12):
                d = (1, 4, 16)[h // 4]
                nq = (S_OWN // d) // 128
                qh, b_qh = qring.next()
                kh, b_kh = kring.next()
                load(qh, qTa_s[h, :, :], b_qh, reads=[sb_q])
                kw_ = (2560, 2560, 3072)[h // 4]
                load(kh[:, 0:kw_], kTa_s[h, :, 0:kw_], b_kh, reads=[sb_k])
                qv = qh.rearrange("p (n d) -> p d n", d=d)
                kv = kh.rearrange("p (n d) -> p d n", d=d)
                aov = ao_s.rearrange("(n d) h c -> d n h c", d=d)
                vav = va_s.rearrange("(n d) c -> d n c", d=d)
                for r in range(d):
                    for i in range(nq):
                        yield attn_item(h, d, r, i, qv, kv, aov, vav, b_qh, b_kh)
        interleave(attn_items(), 8)
        S.barrier()
        arF.p, arB.p = pF_base, pB_base2

        if stop_after == 'D':
            return finish()
        gng = arF.alloc([128, 2048]); b_gng = S.buf('gng')
        load(gng, gng_d[:, :], b_gng)
        NSL = 2
        hs = []
        for sl in range(NSL):
            hs.append(dict(
                DT=arF.alloc([128, 128]), b_DT=S.buf('DT%d' % sl),
                qdt=arF.alloc([128, 4, 128]), b_qdt=S.buf('qdt%d' % sl),
                tmpd=arF.alloc([128, 128]), b_tmpd=S.buf('tmpd%d' % sl),
                Sf=arF.alloc([128, 2, 512]), b_Sf=S.buf('Sf%d' % sl),
                Sb=arF.alloc([128, 2, 512]), b_Sb=S.buf('Sb%d' % sl),
                Sfb=arB.alloc([128, 2, 512]), b_Sfb=S.buf('Sfb%d' % sl),
                Sbb=arB.alloc([128, 2, 512]), b_Sbb=S.buf('Sbb%d' % sl),
                yb=arB.alloc([128, 16, 512]), b_yb=[S.buf('yb%d_%d' % (sl, i)) for i in range(16)],
                qT=arB.alloc([128, 2, S_OWN]), b_qT=S.buf('qTr%d' % sl),
                kT=arB.alloc([128, 2, S_OWN]), b_kT=S.buf('kTr%d' % sl),
            ))
        ktile = Ring(arB, 'ktile', [128, 256], 5)
        vtile = Ring(arB, 'vtile', [128, 512], 5)
        srgt = Ring(arB, 'srgt', [128, 512], 3)
        qsc = Ring(arB, 'qsc', [128, 2, 128], 4)
        innr = Ring(arB, 'innT', [128, 128], 3)
        rgst = Ring(arB, 'rgst', [128, 512], 2)
        ysbr = Ring(arF, 'ysb', [128, 512], 3)
        ynr = Ring(arF, 'yn', [128, 512], 3)
        ytr = Ring(arF, 'yt', [128, 512], 3)
        smring = Ring(arF, 'smE', [128, 32], 6)
        sb_retg = S.buf('retg_s')

        def ret_head(h, H):
            DT, b_DT, qdt, b_qdt, tmpd, b_tmpd = H['DT'], H['b_DT'], H['qdt'], H['b_qdt'], H['tmpd'], H['b_tmpd']
            Sf, b_Sf, Sb_, b_Sb, Sfb, b_Sfb, Sbb, b_Sbb = H['Sf'], H['b_Sf'], H['Sb'], H['b_Sb'], H['Sfb'], H['b_Sfb'], H['Sbb'], H['b_Sbb']
            yb, b_yb, qT, b_qT, kT, b_kT = H['yb'], H['b_yb'], H['qT'], H['b_qT'], H['kT'], H['b_kT']
            lgF = lgt[:, h:h + 1]
            lgB = lgt[:, 4 + h:5 + h]
            TS('dve', tmpd, rtab[:, 0:128], lgF, None, ALU.mult, None, [b_rtab, b_lgt], [b_tmpd])
            STT(tmpd, rtab[:, 128:256], lgB, tmpd, ALU.mult, ALU.add, [b_rtab, b_lgt, b_tmpd], [b_tmpd], part=True)
            ACT(DT, tmpd, AF.Exp, [b_tmpd], [b_DT])
            for c in range(2):
                ACT(qdt[:, c, :], rtab[:, 256:384], AF.Exp, [b_rtab, b_lgt], [b_qdt], scale=lgF, part=(c > 0))
                ACT(qdt[:, 2 + c, :], rtab[:, 384:512], AF.Exp, [b_rtab, b_lgt], [b_qdt], scale=lgB, part=True)
            for c in range(2):
                load(qT[:, c, :], qTr_s[2 * h + c, :, :], b_qT, part=(c > 0), reads=[sb_qr])
                load(kT[:, c, :], kTr_s[2 * h + c, :, :], b_kT, part=(c > 0), reads=[sb_kr])
            yield
            for i in range(NT_ALL - 1, -1, -1):
                kt, b_kt = ktile.next()
                vt, b_vt = vtile.next()
                load(kt, krB_s[i * 128:(i + 1) * 128, h * 256:(h + 1) * 256], b_kt, reads=[sb_krB])
                load(vt, vr_s[i * 128:(i + 1) * 128, h * 512:(h + 1) * 512], b_vt, reads=[sb_vr])
                if i < NT_OWN:
                    qb, b_qb = qsc.next()
                    TT('dve', qb, qT[:, :, i * 128:(i + 1) * 128], qdt[:, 2:4, :], ALU.mult, [b_qT, b_qdt], [b_qb])
                    ps, b_ps = psn()
                    for c in range(2):
                        MM(ps[:, :], qb[:, c, :], Sbb[:, c, :], c == 0, c == 1, [b_qb, b_Sbb], [b_ps])
                    ACT(yb[:, i, :], ps[:, :], AF.Copy, [b_ps], [b_yb[i]])
                if i > 0:
                    pss = []
                    for c in range(2):
                        ps, b_ps = psn()
                        MM(ps[:, :], kt[:, c * 128:(c + 1) * 128], vt, True, True, [b_kt, b_vt], [b_ps])
                        pss.append((ps, b_ps))
                    yield
                    for c in range(2):
                        ps, b_ps = pss[c]
                        if i == NT_ALL - 1:
                            S.op('dve', lambda e, c=c, ps=ps: e.tensor_copy(Sb_[:, c, :], ps[:, :]), reads=[b_ps],
                                 writes=[b_Sb], part=(c > 0))
                        else:
                            STT(Sb_[:, c, :], Sb_[:, c, :], kdt[:, 12 + h:13 + h], ps[:, :], ALU.mult, ALU.add,
                                [b_Sb, b_ps, b_kdt], [b_Sb], part=True)
                    yield
                    if i <= NT_OWN:
                        ACT(Sbb, Sb_, AF.Copy, [b_Sb], [b_Sbb])
                    yield
            for i in range(NT_OWN):
                kt, b_kt = ktile.next()
                vt, b_vt = vtile.next()
                sr, b_sr = srgt.next()
                load(kt, krF_s[i * 128:(i + 1) * 128, h * 256:(h + 1) * 256], b_kt, reads=[sb_krF])
                load(vt, vr_s[i * 128:(i + 1) * 128, h * 512:(h + 1) * 512], b_vt, reads=[sb_vr])
                load(sr, srg_s[i * 128:(i + 1) * 128, h * 512:(h + 1) * 512], b_sr, reads=[sb_srg])
                ps, b_ps = psn()
                for c in range(2):
                    MM(ps[:, 0:128], kT[:, c, i * 128:(i + 1) * 128], qT[:, c, i * 128:(i + 1) * 128], c == 0, c == 1,
                       [b_kT, b_qT], [b_ps])
                pss = []
                if i < NT_OWN - 1:
                    for c in range(2):
                        psu, b_psu = psn()
                        MM(psu[:, :], kt[:, c * 128:(c + 1) * 128], vt, True, True, [b_kt, b_vt], [b_psu])
                        pss.append((psu, b_psu))
                yield
                inn, b_inn = innr.next()
                TT('dve', inn, ps[:, 0:128], DT, ALU.mult, [b_ps, b_DT], [b_inn])
                if i > 0:
                    qf, b_qf = qsc.next()
                    TT('dve', qf, qT[:, :, i * 128:(i + 1) * 128], qdt[:, 0:2, :], ALU.mult, [b_qT, b_qdt], [b_qf])
                yield
                psy, b_psy = psn()
                MM(psy[:, :], inn, vt, True, i == 0, [b_inn, b_vt], [b_psy])
                if i > 0:
                    for c in range(2):
                        MM(psy[:, :], qf[:, c, :], Sfb[:, c, :], False, c == 1, [b_qf, b_Sfb], [b_psy])
                yield
                ysb, b_ysb = ysbr.next()
                TT('dve', ysb, psy[:, :], yb[:, i, :], ALU.add, [b_psy, b_yb[i]], [b_ysb])
                if i < NT_OWN - 1:
                    for c in range(2):
                        psu, b_psu = pss[c]
                        if i == 0:
                            S.op('dve', lambda e, c=c, psu=psu: e.tensor_copy(Sf[:, c, :], psu[:, :]), reads=[b_psu],
                                 writes=[b_Sf], part=(c > 0))
                        else:
                            STT(Sf[:, c, :], Sf[:, c, :], kdt[:, 8 + h:9 + h], psu[:, :], ALU.mult, ALU.add,
                                [b_Sf, b_psu, b_kdt], [b_Sf], part=True)
                    yield
                    ACT(Sfb, Sf, AF.Copy, [b_Sf], [b_Sfb])
                yield
                rstd, nmr, b_sm = ln_stats(ysb, b_ysb, None, smring)
                yield
                yn, b_yn = ynr.next()
                ACT(yn, ysb, AF.Identity, [b_ysb, b_sm], [b_yn], bias=nmr, scale=rstd)
                yield
                yt, b_yt = ytr.next()
                TT('dve', yt, yn, gng[:, h * 512:(h + 1) * 512], ALU.mult, [b_yn, b_gng], [b_yt])
                rg, b_rg = rgst.next()
                TT('pool', rg, yt, sr, ALU.mult, [b_yt, b_sr], [b_rg])
                store(retg_s[i * 128:(i + 1) * 128, h * 512:(h + 1) * 512], rg, sb_retg, b_rg, q='pool')
                yield

        interleave((ret_head(h, hs[h % NSL]) for h in range(4)), NSL)
        S.barrier()
        arF.p, arB.p = pF_base, pB_base2

        if stop_after == 'E':
            return finish()
        Wa = arB.alloc([128, 4, D]); b_Wa = S.buf('Wa')
        Wr = arB.alloc([128, 16, D]); b_Wr = S.buf('Wr')
        Wo = arB.alloc([128, 8, D]); b_Wo = S.buf('Wo')
        load(Wa, wa_d.rearrange("(kc k) n -> k kc n", k=128), b_Wa, cast=True)
        for q4 in range(4):
            load(Wr[:, q4 * 4:(q4 + 1) * 4, :], wr_d[q4 * 512:(q4 + 1) * 512, :].rearrange("(kc k) n -> k kc n", k=128), b_Wr,
                 cast=True, part=(q4 > 0))
        for q4 in range(2):
            load(Wo[:, q4 * 4:(q4 + 1) * 4, :], wo_d[q4 * 512:(q4 + 1) * 512, :].rearrange("(kc k) n -> k kc n", k=128), b_Wo,
                 cast=True, part=(q4 > 0))
        ln1g = arF.alloc([128, D]); b_ln1g = S.buf('ln1g')
        ln1b = arF.alloc([128, D]); b_ln1b = S.buf('ln1b')
        load(ln1g, ln1g_d[:, :], b_ln1g)
        load(ln1b, ln1b_d[:, :], b_ln1b)
        aor = Ring(arF, 'ao', [128, 12, 130], 2)
        xr = Ring(arF, 'xF', [128, D], 2)
        t1r = Ring(arF, 't1F', [128, 512], 3)
        t2r = Ring(arF, 't2F', [128, 512], 3)
        tgr = Ring(arF, 'tgF', [128, D], 2)
        hpr = Ring(arF, 'hpF', [128, D], 2)
        hnr = Ring(arF, 'hnF', [128, D], 1)
        h1r = Ring(arF, 'h1F', [128, D], 2)
        smring = Ring(arF, 'smF', [128, 64], 6)
        rgr = Ring(arB, 'rgF', [128, 2048], 2)
        sgar = Ring(arB, 'sgaF', [128, D], 2)
        sgrr = Ring(arB, 'sgrF', [128, D], 2)
        atr = Ring(arB, 'atF', [128, 512], 2)
        atTr = Ring(arB, 'atTF', [128, 4, 128], 2)
        rgTr = Ring(arB, 'rgTF', [128, 16, 128], 1)
        mgr = Ring(arB, 'mgF', [128, D], 2)
        mgTr = Ring(arB, 'mgTF', [128, 8, 128], 1)
        sb_h1 = S.buf('h1_s')
        def f1_tile(t):
            rows = slice(t * 128, (t + 1) * 128)
            ao, b_ao = aor.next()
            load(ao, ao_s[rows, :, :], b_ao, reads=[sb_ao])
            rgt, b_rgt = rgr.next()
            load(rgt, retg_s[rows, :], b_rgt, reads=[sb_retg])
            sga, b_sga = sgar.next()
            load(sga, sga_s[rows, :], b_sga, reads=[sb_sga])
            sgr, b_sgr = sgrr.next()
            load(sgr, sgr_s[rows, :], b_sgr, reads=[sb_sgr])
            xs, b_xs = xr.next()
            load(xs, x_d[rows, :], b_xs)
            yield
            sm, b_sm = smring.next()
            nm = ao[:, :, 128]
            rs = ao[:, :, 129]
            TT('dve', sm[:, 0:4], nm[:, 0:4], nm[:, 4:8], ALU.min, [b_ao], [b_sm])
            TT('dve', sm[:, 0:4], sm[:, 0:4], nm[:, 8:12], ALU.min, [b_ao, b_sm], [b_sm], part=True)
            for g in range(3):
                TT('dve', sm[:, 4 + 4 * g:8 + 4 * g], nm[:, 4 * g:4 * g + 4], sm[:, 0:4], ALU.subtract, [b_ao, b_sm], [b_sm],
                   part=True)
            ACT(sm[:, 16:28], sm[:, 4:16], AF.Exp, [b_sm], [b_sm], scale=-1.0, part=True)
            TT('dve', sm[:, 28:40], sm[:, 16:28], rs, ALU.mult, [b_sm, b_ao], [b_sm], part=True)
            TT('dve', sm[:, 40:44], sm[:, 28:32], sm[:, 32:36], ALU.add, [b_sm], [b_sm], part=True)
            TT('dve', sm[:, 40:44], sm[:, 40:44], sm[:, 36:40], ALU.add, [b_sm], [b_sm], part=True)
            S.op('dve', lambda e, sm=sm: e.reciprocal(sm[:, 44:48], sm[:, 40:44]), reads=[b_sm], writes=[b_sm], part=True)
            for g in range(3):
                TT('dve', sm[:, 48 + 4 * g:52 + 4 * g], sm[:, 16 + 4 * g:20 + 4 * g], sm[:, 44:48], ALU.mult, [b_sm], [b_sm],
                   part=True)
            yield
            at, b_at = atr.next()
            t1, b_t1 = t1r.next()
            for h4 in range(4):
                TS('dve', t1[:, h4 * 128:(h4 + 1) * 128], ao[:, h4, 0:128], sm[:, 48 + h4:49 + h4], None, ALU.mult, None,
                   [b_ao, b_sm], [b_t1], part=(h4 > 0))
                STT(t1[:, h4 * 128:(h4 + 1) * 128], ao[:, 4 + h4, 0:128], sm[:, 52 + h4:53 + h4],
                    t1[:, h4 * 128:(h4 + 1) * 128], ALU.mult, ALU.add, [b_ao, b_sm, b_t1], [b_t1], part=True)
                STT(at[:, h4 * 128:(h4 + 1) * 128], ao[:, 8 + h4, 0:128], sm[:, 56 + h4:57 + h4],
                    t1[:, h4 * 128:(h4 + 1) * 128], ALU.mult, ALU.add, [b_ao, b_sm, b_t1], [b_at], part=(h4 > 0))
            yield
            ps, b_ps = psn()
            psb = ps[:, :].bitcast(BF16)
            for kc in range(4):
                TR(psb[:, kc * 128:(kc + 1) * 128], at[:, kc * 128:(kc + 1) * 128], identb, [b_at, b_identb], [b_ps], part=(kc > 0))
            atT, b_atT = atTr.next()
            S.op('dve', lambda e, atT=atT, psb=psb: e.tensor_copy(atT, psb[:, 0:512].rearrange("p (a b) -> p a b", a=4)),
                 reads=[b_ps], writes=[b_atT])
            yield
            rgT, b_rgT = rgTr.next()
            for half in range(2):
                ps, b_ps = psn()
                psb = ps[:, :].bitcast(BF16)
                for kc in range(8):
                    TR(psb[:, kc * 128:(kc + 1) * 128], rgt[:, (half * 8 + kc) * 128:(half * 8 + kc + 1) * 128], identb,
                       [b_rgt, b_identb], [b_ps], part=(kc > 0))
                ACT(rgT[:, half * 8:(half + 1) * 8, :], psb.rearrange("p (a b) -> p a b", a=8), AF.Copy, [b_ps], [b_rgT],
                    part=(half > 0))
            mg, b_mg = mgr.next()
            for half in range(2):
                cs = slice(half * 512, (half + 1) * 512)
                psa, b_psa = psn()
                for kc in range(4):
                    MM(psa[:, :], atT[:, kc, :], Wa[:, kc, cs], kc == 0, kc == 3, [b_atT, b_Wa], [b_psa])
                psr, b_psr = psn()
                for kc in range(16):
                    MM(psr[:, :], rgT[:, kc, :], Wr[:, kc, cs], kc == 0, kc == 15, [b_rgT, b_Wr], [b_psr])
                t1, b_t1 = t1r.next()
                t2, b_t2 = t2r.next()
                TT('dve', t1, psa[:, :], sga[:, cs], ALU.mult, [b_psa, b_sga], [b_t1])
                TT('dve', t2, psr[:, :], sgr[:, cs], ALU.mult, [b_psr, b_sgr], [b_t2])
                TT('pool', mg[:, cs], t1, t2, ALU.add, [b_t1, b_t2], [b_mg], part=(half > 0))
            yield
            ps, b_ps = psn()
            psb = ps[:, :].bitcast(BF16)
            for kc in range(8):
                TR(psb[:, kc * 128:(kc + 1) * 128], mg[:, kc * 128:(kc + 1) * 128], identb, [b_mg, b_identb], [b_ps], part=(kc > 0))
            yield
            mgT, b_mgT = mgTr.next()
            ACT(mgT, psb.rearrange("p (a b) -> p a b", a=8), AF.Copy, [b_ps], [b_mgT])
            tg, b_tg = tgr.next()
            for half in range(2):
                cs = slice(half * 512, (half + 1) * 512)
                psy, b_psy = psn()
                for kc in range(8):
                    MM(psy[:, :], mgT[:, kc, :], Wo[:, kc, cs], kc == 0, kc == 7, [b_mgT, b_Wo], [b_psy])
                TT('dve', tg[:, cs], psy[:, :], modbc[:, 2048 + half * 512:2048 + (half + 1) * 512], ALU.mult, [b_psy, b_mod],
                   [b_tg], part=(half > 0))
            yield
            hp, b_hp = hpr.next()
            STT(hp, xs, ALPHA, tg, ALU.mult, ALU.add, [b_xs, b_tg], [b_hp])
            yield
            rstd, nmr, b_sm2 = ln_stats(hp, b_hp, None, smring)
            yield
            hn, b_hn = hnr.next()
            ACT(hn, hp, AF.Identity, [b_hp, b_sm2], [b_hn], bias=nmr, scale=rstd)
            TT('pool', hn, hn, ln1g, ALU.mult, [b_hn, b_ln1g], [b_hn])
            h1, b_h1 = h1r.next()
            TT('pool', h1, hn, ln1b, ALU.add, [b_hn, b_ln1b], [b_h1])
            store(h1_s[rows, :], h1, sb_h1, b_h1, q='pool')
        interleave((f1_tile(t) for t in range(NT_OWN)), 2)
        S.barrier()
        arF.p, arB.p = pF_base, pB_base2

        if stop_after == 'F1':
            return finish()
        u2T = arB.alloc([128, 8, S_OWN]); b_u2T = [S.buf('u2T%d' % t) for t in range(NT_OWN)]
        pB_g = arB.p
        wrt = arF.alloc([128, 8, 36]); b_wrt = S.buf('wrt')
        load(wrt, wrt_d.rearrange("(kc k) n -> k kc n", k=128), b_wrt)
        brt = arF.alloc([128, 36]); b_brt = S.buf('brt')
        load(brt, brt_d[:, :], b_brt)
        h1r = Ring(arF, 'h1G', [128, D], 3)
        hnr = Ring(arF, 'hnG', [128, D], 3)
        tr_ = Ring(arF, 'tG', [128, D], 3)
        u2r = Ring(arF, 'u2G', [128, D], 3)
        u2t32 = Ring(arF, 'u2T32', [128, 8, 128], 3)
        smring = Ring(arF, 'smG', [128, 32], 6)
        rtr = Ring(arF, 'rt', [128, 96], 5)
        u2br = Ring(arB, 'u2b', [128, D], 3)
        def f2_tile(t):
            rows = slice(t * 128, (t + 1) * 128)
            h1, b_h1 = h1r.next()
            load(h1, h1_s[rows, :], b_h1, reads=[sb_h1])
            yield
            rstd, nmr, b_sm = ln_stats(h1, b_h1, None, smring)
            yield
            hn, b_hn = hnr.next()
            ACT(hn, h1, AF.Identity, [b_h1, b_sm], [b_hn], bias=nmr, scale=rstd)
            yield
            tt_, b_tt = tr_.next()
            TT('dve', tt_, hn, modbc[:, 4096:5120], ALU.mult, [b_hn, b_mod], [b_tt])
            yield
            u2, b_u2 = u2r.next()
            TT('pool', u2, tt_, modbc[:, 3072:4096], ALU.add, [b_tt, b_mod], [b_u2])
            yield
            u2b, b_u2b = u2br.next()
            ACT(u2b, u2, AF.Copy, [b_u2], [b_u2b])
            yield
            ps, b_ps = psn()
            psb = ps[:, :].bitcast(BF16)
            for kc in range(8):
                TR(psb[:, kc * 128:(kc + 1) * 128], u2b[:, kc * 128:(kc + 1) * 128], identb, [b_u2b, b_identb], [b_ps], part=(kc > 0))
            yield
            S.op('dve', lambda e, t=t, psb=psb: e.tensor_copy(u2T[:, :, t * 128:(t + 1) * 128],
                                                               psb.rearrange("p (a b) -> p a b", a=8)),
                 reads=[b_ps], writes=[b_u2T[t]])
            yield
            u32, b_u32 = u2t32.next()
            for half in range(2):
                ps, b_ps = psn()
                for kc in range(4):
                    TR(ps[:, kc * 128:(kc + 1) * 128], u2[:, (half * 4 + kc) * 128:(half * 4 + kc + 1) * 128], identf,
                       [b_u2, b_identf], [b_ps], part=(kc > 0))
                ACT(u32[:, half * 4:(half + 1) * 4, :], ps[:, :].rearrange("p (a b) -> p a b", a=4), AF.Copy, [b_ps], [b_u32],
                    part=(half > 0))
            yield
            ps, b_ps = psn()
            for kc in range(8):
                MM(ps[:, 0:36], u32[:, kc, :], wrt[:, kc, :], kc == 0, kc == 7, [b_u32, b_wrt], [b_ps])
            yield
            rt, b_rt = rtr.next()
            TT('dve', rt[:, 0:36], ps[:, 0:36], brt, ALU.add, [b_ps, b_brt], [b_rt])
            S.op('dve', lambda e, rt=rt: e.tensor_reduce(rt[:, 36:37], rt[:, 0:4], AX.X, ALU.max), reads=[b_rt], writes=[b_rt],
                 part=True)
            TS('dve', rt[:, 37:38], rt[:, 36:37], -1.0, None, ALU.mult, None, [b_rt], [b_rt], part=True)
            TS('dve', rt[:, 40:44], rt[:, 0:4], rt[:, 36:37], None, ALU.is_equal, None, [b_rt], [b_rt], part=True)
            yield
            ACT(rt[:, 81:85], rt[:, 0:4], AF.Exp, [b_rt], [b_rt], bias=rt[:, 37:38], scale=1.0, accum=rt[:, 38:39], part=True)
            S.op('dve', lambda e, rt=rt: e.reciprocal(rt[:, 39:40], rt[:, 38:39]), reads=[b_rt], writes=[b_rt], part=True)
            TS('dve', rt[:, 44:52], rt[:, 4:12], rt[:, 40:41], None, ALU.mult, None, [b_rt], [b_rt], part=True)
            for g in range(1, 4):
                STT(rt[:, 44:52], rt[:, 4 + 8 * g:12 + 8 * g], rt[:, 40 + g:41 + g], rt[:, 44:52], ALU.mult, ALU.add, [b_rt], [b_rt],
                    part=True)
            yield
            S.op('dve', lambda e, rt=rt: e.max(rt[:, 52:60], rt[:, 44:52]), reads=[b_rt], writes=[b_rt], part=True)
            TS('dve', rt[:, 60:61], rt[:, 52:53], -1.0, None, ALU.mult, None, [b_rt], [b_rt], part=True)
            yield
            ACT(rt[:, 61:62], rt[:, 53:54], AF.Exp, [b_rt], [b_rt], bias=rt[:, 60:61], scale=1.0, part=True)
            TS('dve', rt[:, 62:63], rt[:, 61:62], 1.0, None, ALU.add, None, [b_rt], [b_rt], part=True)
            S.op('dve', lambda e, rt=rt: e.reciprocal(rt[:, 62:63], rt[:, 62:63]), reads=[b_rt], writes=[b_rt], part=True)
            TT('dve', rt[:, 63:64], rt[:, 62:63], rt[:, 39:40], ALU.mult, [b_rt], [b_rt], part=True)
            TT('dve', rt[:, 64:65], rt[:, 63:64], rt[:, 61:62], ALU.mult, [b_rt], [b_rt], part=True)
            yield
            TS('dve', rt[:, 65:73], rt[:, 44:52], rt[:, 52:53], rt[:, 63:64], ALU.is_equal, ALU.mult, [b_rt], [b_rt], part=True)
            TS('dve', rt[:, 73:81], rt[:, 44:52], rt[:, 53:54], rt[:, 64:65], ALU.is_equal, ALU.mult, [b_rt], [b_rt], part=True)
            TT('dve', rt[:, 73:81], rt[:, 73:81], rt[:, 65:73], ALU.add, [b_rt], [b_rt], part=True)
            for g in range(4):
                TS('dve', gate32[:, t, 8 * g:8 * g + 8], rt[:, 73:81], rt[:, 40 + g:41 + g], None, ALU.mult, None, [b_rt],
                   [b_gate[t]], part=(g > 0))
        interleave((f2_tile(t) for t in range(NT_OWN)), 3)
        S.op('dve', lambda e: e.tensor_copy(gate2bc, modbc[:, 5120:6144]), reads=[b_mod], writes=[b_g2])
        S.barrier()
        arF.p, arB.p = pF_mod, pB_g

        if stop_after == 'F2':
            return finish()
        acc = arF.alloc([128, NT_OWN, D]); b_acc = [[S.buf('acc%d_%d' % (t, hf)) for hf in range(2)] for t in range(NT_OWN)]
        sar = Ring(arF, 'sa', [128, 512], 2)
        wslots = Ring(arB, 'wexp', [128, 4096], 5)
        hTr = Ring(arB, 'hT', [128, 4, 512], 2)
        pend = []

        def issue_e(e):
            res = []
            for nm_, src in (('w1', w1_d[e].rearrange("(kc k) n -> k kc n", k=128)),
                             ('w3', w3_d[e].rearrange("(kc k) n -> k kc n", k=128)),
                             ('w2', w2_d[e].rearrange("(kc k) n -> k kc n", k=128))):
                wt, b_wt = wslots.next()
                if nm_ == 'w2':
                    v = wt.rearrange("p (a b) -> p a b", a=4)
                else:
                    v = wt.rearrange("p (a b) -> p a b", a=8)
                res.append((v, b_wt, src))
            return res

        def do_load(item):
            v, b_wt, src = item
            load(v, src, b_wt, cast=True)

        cur = issue_e(0)
        for it in cur:
            do_load(it)

        def stage_c(e, tb, hT, b_hT, w2t, b_w2):
            for tt in range(4):
                t = tb * 4 + tt
                for hf in range(2):
                    psY, b_psY = psn()
                    for fc in range(4):
                        MM(psY[:, :], hT[:, fc, tt * 128:(tt + 1) * 128], w2t[:, fc, hf * 512:(hf + 1) * 512], fc == 0, fc == 3,
                           [b_hT, b_w2], [b_psY])
                    dst = acc[:, t, hf * 512:(hf + 1) * 512]
                    if e == 0:
                        TS('dve', dst, psY[:, :], gate32[:, t, e:e + 1], None, ALU.mult, None, [b_psY, b_gate[t]],
                           [b_acc[t][hf]])
                    else:
                        STT(dst, psY[:, :], gate32[:, t, e:e + 1], dst, ALU.mult, ALU.add, [b_psY, b_gate[t], b_acc[t][hf]],
                            [b_acc[t][hf]])

        pend_c = None
        for e in range(32):
            (w1t, b_w1, _), (w3t, b_w3, _), (w2t, b_w2, _) = cur
            nxt = issue_e(e + 1) if e + 1 < 32 else None
            for tb in range(4):
                hT, b_hT = hTr.next()
                toks = slice(tb * 512, (tb + 1) * 512)
                for fc in range(4):
                    psA, b_psA = psn()
                    for kc in range(8):
                        MM(psA[:, :], w1t[:, kc, fc * 128:(fc + 1) * 128], u2T[:, kc, toks], kc == 0, kc == 7,
                           [b_w1] + b_u2T[tb * 4:tb * 4 + 4], [b_psA])
                    psBk, b_psBk = psn()
                    for kc in range(8):
                        MM(psBk[:, :], w3t[:, kc, fc * 128:(fc + 1) * 128], u2T[:, kc, toks], kc == 0, kc == 7,
                           [b_w3] + b_u2T[tb * 4:tb * 4 + 4], [b_psBk])
                    sa, b_sa = sar.next()
                    ACT(sa, psA[:, :], AF.Silu, [b_psA], [b_sa])
                    TT('dve', hT[:, fc, :], sa, psBk[:, :], ALU.mult, [b_sa, b_psBk], [b_hT], part=(fc > 0))
                if pend_c is not None:
                    stage_c(*pend_c)
                pend_c = (e, tb, hT, b_hT, w2t, b_w2)
                if tb == 0 and nxt:
                    do_load(nxt[0])
                    do_load(nxt[1])
            if nxt:
                do_load(nxt[2])
            cur = nxt
        stage_c(*pend_c)

        if stop_after == 'G':
            return finish()
        ln2g = arF.alloc([128, D]); b_ln2g = S.buf('ln2g')
        ln2b = arF.alloc([128, D]); b_ln2b = S.buf('ln2b')
        load(ln2g, ln2g_d[:, :], b_ln2g)
        load(ln2b, ln2b_d[:, :], b_ln2b)
        h1r = Ring(arF, 'h1H', [128, D], 2)
        outr = Ring(arF, 'oH', [128, D], 2)
        smring = Ring(arF, 'smH', [128, 32], 4)
        b_out = S.buf('out')
        for t in range(NT_OWN):
            rows = slice(t * 128, (t + 1) * 128)
            h1, b_h1 = h1r.next()
            load(h1, h1_s[rows, :], b_h1, reads=[sb_h1])
            ot, b_ot = outr.next()
            a_t = acc[:, t, :]
            ba = b_acc[t]
            TT('dve', ot, a_t, gate2bc, ALU.mult, ba + [b_g2], [b_ot])
            for hf in range(2):
                cs = slice(hf * 512, (hf + 1) * 512)
                STT(a_t[:, cs], h1[:, cs], ALPHA, ot[:, cs], ALU.mult, ALU.add, [b_h1, b_ot], [ba[hf]])
            sm, b_sm = smring.next()
            for j in range(2):
                S.op('dve', lambda e, j=j, sm=sm, a_t=a_t: e.bn_stats(sm[:, 6 * j:6 * j + 6], a_t[:, 512 * j:512 * (j + 1)]),
                     reads=[ba[j]], writes=[b_sm], part=(j > 0))
            S.op('dve', lambda e, sm=sm: e.bn_aggr(sm[:, 12:14], sm[:, 0:12]), reads=[b_sm], writes=[b_sm], part=True)
            ACT(sm[:, 14:15], sm[:, 13:14], AF.Sqrt, [b_sm, b_epsc], [b_sm], bias=epsc[:, 0:1], scale=1.0, part=True)
            S.op('dve', lambda e, sm=sm: e.reciprocal(sm[:, 15:16], sm[:, 14:15]), reads=[b_sm], writes=[b_sm], part=True)
            STT(sm[:, 16:17], sm[:, 12:13], -1.0, sm[:, 15:16], ALU.mult, ALU.mult, [b_sm], [b_sm], part=True)
            ACT(ot, a_t, AF.Identity, ba + [b_sm], [b_ot], bias=sm[:, 16:17], scale=sm[:, 15:16])
            TT('pool', ot, ot, ln2g, ALU.mult, [b_ot, b_ln2g], [b_ot])
            TT('pool', ot, ot, ln2b, ALU.add, [b_ot, b_ln2b], [b_ot])
            S.op('pool', lambda e, ot=ot, rows=rows: e.dma_start(out=out_d[rows, :], in_=ot), reads=[b_ot], writes=[b_out],
                 part=True, dma=True, sembuf=b_ot)
        S.barrier()
        blk = st.enter_context(nc.Block())
        S.emit(blk)
    return nc


_NC_CACHE = {}


def _host_tables():
    import ml_dtypes
    slopes = np.exp2(-8.0 * np.arange(1, 13, dtype=np.float32) / 12.0).astype(np.float32)
    i = np.arange(128)[:, None]
    j = np.arange(128)[None, :]
    ab = np.zeros((128, 12, 4, 128), np.float32)
    for h in range(12):
        d = (1, 4, 16)[h // 4]
        dA = j - 64 - i
        dB = j + 64 - i
        dF = j - i
        tA = np.where(np.abs(dA) <= 64, -slopes[h] * d * np.abs(dA), NEG)
        tB = np.where(np.abs(dB) <= 64, -slopes[h] * d * np.abs(dB), NEG)
        tF = np.where((np.abs(dF) <= 64) & (j < 64), -slopes[h] * d * np.abs(dF), NEG)
        ab[:, h, 0] = tA
        ab[:, h, 1] = tB
        ab[:, h, 2] = tF
        ab[:, h, 3] = tB
    ab = ab.reshape(128, 12 * 4 * 128).astype(np.float32)
    m = np.arange(128, dtype=np.float32)[:, None]
    n = np.arange(128, dtype=np.float32)[None, :]
    rt = np.zeros((128, 4 * 128 + 8), np.float32)
    rt[:, 0:128] = np.maximum(n - m, 0)
    rt[:, 128:256] = np.maximum(m - n, 0)
    rt[:, 256:384] = n + 1
    rt[:, 384:512] = 128 - n
    rt[:, 512] = 127 - m[:, 0]
    rt[:, 513] = m[:, 0]
    rt[:, 514] = 128
    identf = np.eye(128, dtype=np.float32)
    identb = np.eye(128, dtype=np.float32).astype(ml_dtypes.bfloat16)
    return ab, rt, identf, identb


def _prep(x, c, w_ada, b_ada, w_in, w_attn_out, ret_decay_fwd, ret_decay_bwd, ret_gn_gain,
          w_ret_out, w_out, ln1_gain, ln1_bias, w_coarse, b_coarse, w_fine, b_fine,
          w1, w3, w2, ln2_gain, ln2_bias, ne=32, cores=range(8)):
    f = lambda a: np.ascontiguousarray(np.asarray(a, dtype=np.float32))
    x = f(x); c = f(c)
    ab, rt, identf, identb = _host_tables()
    rep = lambda v: np.ascontiguousarray(np.broadcast_to(f(v).reshape(1, -1), (128, f(v).size)))
    wrt = np.ascontiguousarray(np.concatenate([f(w_coarse)[0], f(w_fine)[0].transpose(1, 0, 2).reshape(D, 32)], axis=1))
    brt = rep(np.concatenate([f(b_coarse)[0].reshape(-1), f(b_fine)[0].reshape(-1)]))
    shared = {
        "w_ada": f(w_ada)[0], "bada": rep(b_ada[0]), "w_in": f(w_in)[0], "w_attn_out": f(w_attn_out)[0],
        "w_ret_out": f(w_ret_out)[0], "w_out": f(w_out)[0], "gng": rep(ret_gn_gain[0]),
        "ln1g": rep(ln1_gain[0]), "ln1b": rep(ln1_bias[0]), "ln2g": rep(ln2_gain[0]), "ln2b": rep(ln2_bias[0]),
        "wrt": wrt, "brt": brt, "w1": f(w1)[0][:ne], "w3": f(w3)[0][:ne], "w2": f(w2)[0][:ne],
        "abias": ab, "rtab": rt, "identf": identf, "identb": identb,
    }
    lf = f(ret_decay_fwd)[0]
    lb = f(ret_decay_bwd)[0]
    in_maps = []
    for core in cores:
        b, half = core // 2, core % 2
        xc = x[b] if half == 0 else x[b, ::-1]
        cb = np.ascontiguousarray(np.broadcast_to(c[b].reshape(8, 128).T[:, :, None], (128, 8, 128)))
        rl = np.concatenate([lf, lb]) if half == 0 else np.concatenate([lb, lf])
        m = dict(shared)
        m["x"] = np.ascontiguousarray(xc)
        m["cbc"] = cb
        m["retlog"] = rep(rl)
        in_maps.append(m)
    return in_maps


def kernel(**inputs):
    if 'nc' not in _NC_CACHE:
        _NC_CACHE['nc'] = build_nc()
    nc = _NC_CACHE['nc']
    in_maps = _prep(**inputs)
    res = run_bass_kernel_spmd(nc, in_maps, core_ids=list(range(8)))
    out = np.zeros((4, S_ALL, D), np.float32)
    for core in range(8):
        b, half = core // 2, core % 2
        o = np.asarray(res.results[core]["out"], dtype=np.float32)
        if half == 0:
            out[b, :S_OWN] = o
        else:
            out[b, S_OWN:] = o[::-1]
    return out
```
